# Optimizing a Trainium2 kernel written in Bass

```python
import math
import jax, jax.numpy as jnp
from jax import lax
import numpy as np

D_MODEL = 2048
BATCH = 4
SEQ = 2048
DEPTH = 4

D_CONV = 1024
CONV_WIDTH = 31
SB_HEADS = 8
SB_HEAD_DIM = 128
SB_WIDTH = SB_HEADS * SB_HEAD_DIM
NSA_HEADS = 8
NSA_KV_HEADS = 2
NSA_HEAD_DIM = 128
NSA_WIDTH = NSA_HEADS * NSA_HEAD_DIM
NSA_KV_WIDTH = NSA_KV_HEADS * NSA_HEAD_DIM
CMP_LEN = 32
CMP_STRIDE = 16
SLC_LEN = 64
SLC_TOP_N = 16
N_LOCAL_BLOCKS = 2
WINDOW = 512
FORCE_SCORE = 1e4
REL_BUCKETS = 32
REL_MAX_DIST = 128
D_FF = 5632
N_EXPERTS = 8
TOP_K = 2
D_FF_EXPERT = 2816
Q_BLOCK = 128
SLC_Q_BLOCK = 32
EPS = 1e-6
NEG_INF = -1e30
TINY = 1e-20
IN_SIZES = (2 * D_CONV, SB_WIDTH, SB_WIDTH, SB_WIDTH, NSA_WIDTH, NSA_KV_WIDTH, NSA_KV_WIDTH, NSA_KV_WIDTH, NSA_KV_WIDTH, NSA_KV_WIDTH, NSA_KV_WIDTH, 3 * NSA_HEADS, 3 * D_MODEL)

kernel_name = "hybrid_conv_stickbreak_nsa_moe_block"


def rms_norm(x, g):
    xf = x.astype(jnp.float32)
    y = xf * lax.rsqrt(jnp.mean(xf * xf, axis=-1, keepdims=True) + EPS)
    return (y * g.astype(jnp.float32)).astype(x.dtype)


def layer_norm(x, g, b):
    xf = x.astype(jnp.float32)
    mu = jnp.mean(xf, axis=-1, keepdims=True)
    var = jnp.mean(jnp.square(xf - mu), axis=-1, keepdims=True)
    return ((xf - mu) * lax.rsqrt(var + EPS) * g.astype(jnp.float32) + b.astype(jnp.float32)).astype(x.dtype)


def masked_softmax(logits, mask):
    logits = jnp.where(mask, logits.astype(jnp.float32), NEG_INF)
    m = jnp.max(logits, axis=-1, keepdims=True)
    p = jnp.where(mask, jnp.exp(logits - m), 0.0)
    return p / jnp.maximum(jnp.sum(p, axis=-1, keepdims=True), TINY)


def rel_bucket(dist):
    n = jnp.maximum(dist, 0)
    max_exact = REL_BUCKETS // 2
    nf = jnp.maximum(n, 1).astype(jnp.float32)
    large = max_exact + (jnp.log(nf / max_exact) / math.log(REL_MAX_DIST / max_exact) * (REL_BUCKETS - max_exact)).astype(jnp.int32)
    large = jnp.minimum(large, REL_BUCKETS - 1)
    return jnp.where(n < max_exact, n, large)


def to_heads(t, n):
    b, s, _ = t.shape
    return t.reshape(b, s, n, -1).transpose(0, 2, 1, 3)


def merge_heads(t):
    b, h, s, d = t.shape
    return t.transpose(0, 2, 1, 3).reshape(b, s, h * d)


def split_points():
    return np.cumsum(np.array(IN_SIZES))[:-1].tolist()


def conv_module(glu_in, conv_w, conv_b, ln_g, ln_b, w_out):
    a, gate = jnp.split(glu_in, 2, axis=-1)
    u = a * jax.nn.sigmoid(gate)
    u = lax.conv_general_dilated(u, conv_w[:, None, :], window_strides=(1,), padding=[(CONV_WIDTH - 1, 0)],
                                 dimension_numbers=('NWC', 'WIO', 'NWC'), feature_group_count=D_CONV) + conv_b
    u = jax.nn.silu(layer_norm(u, ln_g, ln_b))
    return u @ w_out


def stick_breaking_attention(q, k, v):
    b, h, s, d = q.shape
    nblk = s // Q_BLOCK
    scale = d ** -0.5
    kpos = jnp.arange(s)
    q_blocks = q.reshape(b, h, nblk, Q_BLOCK, d).transpose(2, 0, 1, 3, 4)

    def block(args):
        qb, i = args
        tpos = i * Q_BLOCK + jnp.arange(Q_BLOCK)
        before = kpos[None, :] < tpos[:, None]
        z = jnp.einsum('bhqd,bhkd->bhqk', qb, k).astype(jnp.float32) * scale
        log_beta = jax.nn.log_sigmoid(z)
        log_keep = jnp.where(before, jax.nn.log_sigmoid(-z), 0.0)
        log_survive = lax.cumsum(log_keep, axis=3, reverse=True) - log_keep
        a = jnp.where(before, jnp.exp(log_beta + log_survive), 0.0)
        return jnp.einsum('bhqk,bhkd->bhqd', a.astype(v.dtype), v)

    out = lax.map(block, (q_blocks, jnp.arange(nblk)))
    return out.transpose(1, 2, 0, 3, 4).reshape(b, h, s, d)


def compress_blocks(kv, pos, w):
    s = kv.shape[2]
    nc = (s - CMP_LEN) // CMP_STRIDE + 1
    idx = np.arange(nc)[:, None] * CMP_STRIDE + np.arange(CMP_LEN)[None, :]
    blocks = kv[:, :, idx, :] + pos
    return blocks.reshape(blocks.shape[0], blocks.shape[1], nc, -1) @ w


def nsa_attention(q, kc, vc, ks, vs, kw, vw, gate_logits, cmp_pos_k, cmp_pos_v, cmp_wk, cmp_wv, q_g, kc_g, ks_g, kw_g, rel_bias):
    b, s, _ = q.shape
    g_n, r_n, d = NSA_KV_HEADS, NSA_HEADS // NSA_KV_HEADS, NSA_HEAD_DIM
    scale = d ** -0.5
    qh = rms_norm(to_heads(q, NSA_HEADS), q_g).reshape(b, g_n, r_n, s, d)
    tpos = jnp.arange(s)
    table = rel_bias.T.reshape(g_n, r_n, REL_BUCKETS)
    table_g = table.transpose(0, 2, 1)

    kcb = rms_norm(compress_blocks(to_heads(kc, g_n), cmp_pos_k, cmp_wk), kc_g)
    vcb = compress_blocks(to_heads(vc, g_n), cmp_pos_v, cmp_wv)
    nc = kcb.shape[2]
    cend = jnp.arange(nc) * CMP_STRIDE + CMP_LEN - 1
    dist_c = tpos[:, None] - cend[None, :]
    logit_c = jnp.einsum('bgrsd,bgcd->bgrsc', qh, kcb).astype(jnp.float32) * scale + table[:, :, rel_bucket(dist_c)]
    p_c = masked_softmax(logit_c, dist_c >= 0)
    o_c = jnp.einsum('bgrsc,bgcd->bgrsd', p_c.astype(vcb.dtype), vcb)

    nsel = s // SLC_LEN
    ci = np.arange(nc)[:, None]
    sj = np.arange(nsel)[None, :]
    overlap = (ci * CMP_STRIDE <= sj * SLC_LEN + SLC_LEN - 1) & (ci * CMP_STRIDE + CMP_LEN - 1 >= sj * SLC_LEN)
    imp = jnp.einsum('bgrsc,cn->bgsn', p_c, jnp.asarray(overlap, jnp.float32))
    tb = tpos // SLC_LEN
    jb = jnp.arange(nsel)
    causal_blk = jb[None, :] <= tb[:, None]
    forced = (jb[None, :] == 0) | (causal_blk & (jb[None, :] > tb[:, None] - N_LOCAL_BLOCKS))
    score = jnp.where(forced, FORCE_SCORE, jnp.where(causal_blk, imp, -FORCE_SCORE))
    n_top = min(SLC_TOP_N, nsel)
    top_score, top_idx = lax.top_k(score, n_top)
    top_ok = top_score > -0.5 * FORCE_SCORE

    ks_blk = rms_norm(to_heads(ks, g_n), ks_g).reshape(b, g_n, nsel, SLC_LEN, d)
    vs_blk = to_heads(vs, g_n).reshape(b, g_n, nsel, SLC_LEN, d)
    nq = s // SLC_Q_BLOCK
    bi = jnp.arange(b)[:, None, None, None]
    gi = jnp.arange(g_n)[None, :, None, None]

    def sel_chunk(args):
        qc, idx, ok, i = args
        t = i * SLC_Q_BLOCK + jnp.arange(SLC_Q_BLOCK)
        kg = ks_blk[bi, gi, idx]
        vg = vs_blk[bi, gi, idx]
        tok = idx[..., None] * SLC_LEN + jnp.arange(SLC_LEN)
        dist = t[None, None, :, None, None] - tok
        mask = (dist >= 0) & ok[..., None]
        bias = jnp.moveaxis(table_g[gi[..., None], rel_bucket(dist)], -1, 2)
        logits = jnp.einsum('bgrqd,bgqnld->bgrqnl', qc, kg).astype(jnp.float32) * scale + bias
        p = masked_softmax(logits.reshape(b, g_n, r_n, SLC_Q_BLOCK, -1), mask.reshape(b, g_n, 1, SLC_Q_BLOCK, -1))
        return jnp.einsum('bgrqm,bgqmd->bgrqd', p.astype(vg.dtype), vg.reshape(b, g_n, SLC_Q_BLOCK, -1, d))

    q_chunks = qh.reshape(b, g_n, r_n, nq, SLC_Q_BLOCK, d).transpose(3, 0, 1, 2, 4, 5)
    idx_chunks = top_idx.reshape(b, g_n, nq, SLC_Q_BLOCK, n_top).transpose(2, 0, 1, 3, 4)
    ok_chunks = top_ok.reshape(b, g_n, nq, SLC_Q_BLOCK, n_top).transpose(2, 0, 1, 3, 4)
    o_s = lax.map(sel_chunk, (q_chunks, idx_chunks, ok_chunks, jnp.arange(nq)))
    o_s = o_s.transpose(1, 2, 3, 0, 4, 5).reshape(b, g_n, r_n, s, d)

    pad = ((0, 0), (0, 0), (WINDOW, 0), (0, 0))
    kw_p = jnp.pad(rms_norm(to_heads(kw, g_n), kw_g), pad)
    vw_p = jnp.pad(to_heads(vw, g_n), pad)
    span = WINDOW + Q_BLOCK
    nb = s // Q_BLOCK

    def win_block(args):
        qb, i = args
        start = i * Q_BLOCK
        kb = lax.dynamic_slice_in_dim(kw_p, start, span, axis=2)
        vb = lax.dynamic_slice_in_dim(vw_p, start, span, axis=2)
        t = start + jnp.arange(Q_BLOCK)
        spos = start - WINDOW + jnp.arange(span)
        dist = t[:, None] - spos[None, :]
        mask = (dist >= 0) & (dist < WINDOW) & (spos[None, :] >= 0)
        logits = jnp.einsum('bgrqd,bgkd->bgrqk', qb, kb).astype(jnp.float32) * scale + table[:, :, rel_bucket(dist)]
        p = masked_softmax(logits, mask)
        return jnp.einsum('bgrqk,bgkd->bgrqd', p.astype(vb.dtype), vb)

    q_blocks = qh.reshape(b, g_n, r_n, nb, Q_BLOCK, d).transpose(3, 0, 1, 2, 4, 5)
    o_w = lax.map(win_block, (q_blocks, jnp.arange(nb)))
    o_w = o_w.transpose(1, 2, 3, 0, 4, 5).reshape(b, g_n, r_n, s, d)

    gates = jax.nn.sigmoid(gate_logits.astype(jnp.float32)).reshape(b, s, 3, g_n, r_n).transpose(2, 0, 3, 4, 1)[..., None].astype(q.dtype)
    o = gates[0] * o_c + gates[1] * o_s + gates[2] * o_w
    return o.transpose(0, 3, 1, 2, 4).reshape(b, s, NSA_WIDTH)


def hybrid_mixer(h, w_in, conv_w, conv_b, conv_ln_g, conv_ln_b, w_conv_out, w_sb_out, cmp_pos_k, cmp_pos_v, cmp_wk, cmp_wv,
                 q_g, kc_g, ks_g, kw_g, w_nsa_out, w_o, rel_bias):
    (glu_in, sb_q, sb_k, sb_v, nq, nkc, nvc, nks, nvs, nkw, nvw, nsa_gate, merge_gate) = jnp.split(h @ w_in, split_points(), axis=-1)
    y_conv = conv_module(glu_in, conv_w, conv_b, conv_ln_g, conv_ln_b, w_conv_out)
    y_sb = merge_heads(stick_breaking_attention(to_heads(sb_q, SB_HEADS), to_heads(sb_k, SB_HEADS), to_heads(sb_v, SB_HEADS))) @ w_sb_out
    y_nsa = nsa_attention(nq, nkc, nvc, nks, nvs, nkw, nvw, nsa_gate, cmp_pos_k, cmp_pos_v, cmp_wk, cmp_wv,
                          q_g, kc_g, ks_g, kw_g, rel_bias) @ w_nsa_out
    g_conv, g_sb, g_nsa = jnp.split(jax.nn.sigmoid(merge_gate), 3, axis=-1)
    return (g_conv * y_conv + g_sb * y_sb + g_nsa * y_nsa) @ w_o


def swiglu(t, w1, w3, w2):
    return (jax.nn.silu(t @ w1) * (t @ w3)) @ w2


def moe_swiglu(h, w_router, b_router, w1, w3, w2):
    b, s, dm = h.shape
    t = h.reshape(-1, dm)
    logits = (t @ w_router).astype(jnp.float32) + b_router.astype(jnp.float32)
    top_vals, top_idx = lax.top_k(logits, TOP_K)
    top_w = jax.nn.softmax(top_vals, axis=-1)
    combine = jnp.sum(jax.nn.one_hot(top_idx, N_EXPERTS, dtype=jnp.float32) * top_w[..., None], axis=1).astype(h.dtype)
    out = jnp.zeros_like(t)
    for e in range(N_EXPERTS):
        out = out + combine[:, e:e + 1] * swiglu(t, w1[e], w3[e], w2[e])
    return out.reshape(b, s, dm)


def setup_inputs(seed: int = 0) -> dict:
    key = jax.random.key(seed)
    k = jax.random.split(key, 32)
    n_dense = (DEPTH + 1) // 2
    n_moe = DEPTH // 2
    n_in = int(sum(IN_SIZES))
    d = NSA_HEAD_DIM

    def nrm(i, shape, scale):
        return jax.random.normal(k[i], shape, jnp.float32) * scale

    def gain(i, shape):
        return 1.0 + nrm(i, shape, 0.02)

    return {
        "x": nrm(0, (BATCH, SEQ, D_MODEL), 1.0),
        "c": nrm(1, (BATCH, D_MODEL), 1.0),
        "w_ada": nrm(2, (DEPTH, D_MODEL, 6 * D_MODEL), 0.5 * D_MODEL ** -0.5),
        "b_ada": nrm(3, (DEPTH, 6 * D_MODEL), 0.01),
        "g_mix": gain(4, (DEPTH, D_MODEL)),
        "g_ffn": gain(5, (DEPTH, D_MODEL)),
        "w_in": nrm(6, (DEPTH, D_MODEL, n_in), D_MODEL ** -0.5),
        "conv_w": nrm(7, (DEPTH, CONV_WIDTH, D_CONV), CONV_WIDTH ** -0.5),
        "conv_b": nrm(8, (DEPTH, D_CONV), 0.01),
        "conv_ln_g": gain(9, (DEPTH, D_CONV)),
        "conv_ln_b": nrm(10, (DEPTH, D_CONV), 0.01),
        "w_conv_out": nrm(11, (DEPTH, D_CONV, D_MODEL), D_CONV ** -0.5),
        "w_sb_out": nrm(12, (DEPTH, SB_WIDTH, D_MODEL), SB_WIDTH ** -0.5),
        "nsa_cmp_pos_k": nrm(13, (DEPTH, CMP_LEN, d), 0.02),
        "nsa_cmp_pos_v": nrm(14, (DEPTH, CMP_LEN, d), 0.02),
        "nsa_cmp_wk": nrm(15, (DEPTH, CMP_LEN * d, d), (CMP_LEN * d) ** -0.5),
        "nsa_cmp_wv": nrm(16, (DEPTH, CMP_LEN * d, d), (CMP_LEN * d) ** -0.5),
        "nsa_q_g": gain(17, (DEPTH, d)),
        "nsa_kc_g": gain(18, (DEPTH, d)),
        "nsa_ks_g": gain(19, (DEPTH, d)),
        "nsa_kw_g": gain(20, (DEPTH, d)),
        "w_nsa_out": nrm(21, (DEPTH, NSA_WIDTH, D_MODEL), NSA_WIDTH ** -0.5),
        "w_o": nrm(22, (DEPTH, D_MODEL, D_MODEL), D_MODEL ** -0.5),
        "rel_bias": nrm(23, (REL_BUCKETS, NSA_HEADS), 0.1),
        "ffn_w1": nrm(24, (n_dense, D_MODEL, D_FF), D_MODEL ** -0.5),
        "ffn_w3": nrm(25, (n_dense, D_MODEL, D_FF), D_MODEL ** -0.5),
        "ffn_w2": nrm(26, (n_dense, D_FF, D_MODEL), D_FF ** -0.5),
        "moe_router": nrm(27, (n_moe, D_MODEL, N_EXPERTS), D_MODEL ** -0.5),
        "moe_router_b": nrm(28, (n_moe, N_EXPERTS), 0.01),
        "moe_w1": nrm(29, (n_moe, N_EXPERTS, D_MODEL, D_FF_EXPERT), D_MODEL ** -0.5),
        "moe_w3": nrm(30, (n_moe, N_EXPERTS, D_MODEL, D_FF_EXPERT), D_MODEL ** -0.5),
        "moe_w2": nrm(31, (n_moe, N_EXPERTS, D_FF_EXPERT, D_MODEL), D_FF_EXPERT ** -0.5),
    }


def reference(x, c, w_ada, b_ada, g_mix, g_ffn, w_in, conv_w, conv_b, conv_ln_g, conv_ln_b, w_conv_out, w_sb_out,
              nsa_cmp_pos_k, nsa_cmp_pos_v, nsa_cmp_wk, nsa_cmp_wv, nsa_q_g, nsa_kc_g, nsa_ks_g, nsa_kw_g, w_nsa_out, w_o,
              rel_bias, ffn_w1, ffn_w3, ffn_w2, moe_router, moe_router_b, moe_w1, moe_w3, moe_w2):
    c_act = jax.nn.silu(c)
    for layer in range(DEPTH):
        mod = (c_act @ w_ada[layer] + b_ada[layer])[:, None, :]
        sh_mix, sc_mix, ga_mix, sh_ffn, sc_ffn, ga_ffn = jnp.split(mod, 6, axis=-1)
        h = rms_norm(x, g_mix[layer]) * (1 + sc_mix) + sh_mix
        y = hybrid_mixer(h, w_in[layer], conv_w[layer], conv_b[layer], conv_ln_g[layer], conv_ln_b[layer], w_conv_out[layer],
                         w_sb_out[layer], nsa_cmp_pos_k[layer], nsa_cmp_pos_v[layer], nsa_cmp_wk[layer], nsa_cmp_wv[layer],
                         nsa_q_g[layer], nsa_kc_g[layer], nsa_ks_g[layer], nsa_kw_g[layer], w_nsa_out[layer], w_o[layer], rel_bias)
        x = x + ga_mix * y
        h = rms_norm(x, g_ffn[layer]) * (1 + sc_ffn) + sh_ffn
        i = layer // 2
        if layer % 2 == 0:
            y = swiglu(h, ffn_w1[i], ffn_w3[i], ffn_w2[i])
        else:
            y = moe_swiglu(h, moe_router[i], moe_router_b[i], moe_w1[i], moe_w3[i], moe_w2[i])
        x = x + ga_ffn * y
    return x
```

```python
import math
from contextlib import ExitStack

import numpy as np
import ml_dtypes
import concourse.bass as bass
import concourse.mybir as mybir
from concourse.bass_utils import run_bass_kernel_spmd

F32 = mybir.dt.float32
BF16 = mybir.dt.bfloat16
AF = mybir.ActivationFunctionType
ALU = mybir.AluOpType

L = 4
D = 2048
S = 2048
NIN = 13848
DFF = 5632
NE = 8
DFE = 2816
EPS = 1e-6
NEG = -30000.0
C_GLU, C_SBQ, C_SBK, C_SBV, C_NQ, C_KC, C_VC, C_KS, C_VS, C_KW, C_VW, C_NG, C_MG = (
    0, 2048, 3072, 4096, 5120, 6144, 6400, 6656, 6912, 7168, 7424, 7680, 7704)
V_BADA, V_GMIX, V_GFFN, V_CB, V_LNG, V_LNB, V_CW, V_QG, V_KCG, V_KSG, V_KWG, V_PK, V_PV, NV = (
    0, 96, 112, 128, 136, 144, 152, 400, 401, 402, 403, 404, 436, 468)


class Buf:
    __slots__ = ("name", "w", "r")

    def __init__(self, name=""):
        self.name = name
        self.w = {}
        self.r = {}


class Prog:
    def __init__(self, nc, stack, n_dma_sems=(24, 24)):
        self.nc = nc
        self.engs = {"pe": nc.tensor, "dve": nc.vector, "act": nc.scalar,
                     "pool": nc.gpsimd, "sp": nc.sync}
        self.sems = {}
        self.cnt = {}
        for e in self.engs:
            self.sems[e] = stack.enter_context(nc.semaphore("c_" + e))
            self.cnt[e] = 0
        self.dq = {}
        for q, n in zip(("sp", "pool"), n_dma_sems):
            lst = []
            for i in range(n):
                key = f"d_{q}{i}"
                self.sems[key] = stack.enter_context(nc.semaphore(key))
                self.cnt[key] = 0
                lst.append(key)
            self.dq[q] = [lst, 0]
        self.waited = {e: {} for e in self.engs}
        self.n_wait = 0
        self.n_ins = 0

    def _wait(self, eng, key, val):
        if val <= 0:
            return
        w = self.waited[eng]
        if w.get(key, 0) >= val:
            return
        self.engs[eng].wait_ge(self.sems[key], val)
        w[key] = val
        self.n_wait += 1

    def _deps(self, eng, R, W, A):
        need = {}

        def add(d):
            for k, v in d.items():
                if need.get(k, 0) < v:
                    need[k] = v
        for b in R:
            add(b.w)
        for b in W:
            add(b.w)
            add(b.r)
        for b in A:
            add(b.r)
        for k, v in need.items():
            if eng == "pe" and k == "pe":
                continue
            self._wait(eng, k, v)

    def _commit(self, key, val, R, W, A):
        for b in R:
            if b.r.get(key, 0) < val:
                b.r[key] = val
        for b in W:
            b.w = {key: val}
            b.r = {}
        for b in A:
            if b.w.get(key, 0) < val:
                b.w[key] = val

    def I(self, eng, fn, *args, R=(), W=(), A=(), sig=True, **kw):
        self._deps(eng, R, W, A)
        ins = getattr(self.engs[eng], fn)(*args, **kw)
        self.n_ins += 1
        if sig:
            self.cnt[eng] += 1
            ins.then_inc(self.sems[eng], 1)
            val = self.cnt[eng]
        else:
            val = self.cnt[eng] + 1
        self._commit(eng, val, R, W, A)
        return ins

    def dma(self, q, out, in_, R=(), W=(), A=(), **kw):
        self._deps(q, R, W, A)
        lst, idx = self.dq[q]
        key = lst[idx % len(lst)]
        self.dq[q][1] = idx + 1
        self._wait(q, key, self.cnt[key])
        ins = self.engs[q].dma_start(out=out, in_=in_, **kw)
        self.cnt[key] += 16
        ins.then_inc(self.sems[key], 16)
        self.n_ins += 1
        self._commit(key, self.cnt[key], R, W, A)
        return ins

    def finish(self, eng, bufs):
        for b in bufs:
            for k, v in b.w.items():
                self._wait(eng, k, v)

    def drain_dmas(self):
        for q in self.dq:
            for key in self.dq[q][0]:
                self._wait(q, key, self.cnt[key])


class Rot:
    def __init__(self, tiles, name):
        self.t = tiles
        self.b = [Buf(f"{name}{i}") for i in range(len(tiles))]
        self.i = 0

    def next(self):
        j = self.i % len(self.t)
        self.i += 1
        return self.t[j], self.b[j]


def _rel_bucket(dist):
    n = np.maximum(dist, 0)
    nf = np.maximum(n, 1).astype(np.float32)
    large = 16 + (np.log(nf / np.float32(16)) / np.float32(math.log(128 / 16)) * np.float32(16)).astype(np.int32)
    large = np.minimum(large, 31)
    return np.where(n < 16, n, large)


def _host_consts(rel_bias):
    c = {}
    c["ident"] = np.eye(128, dtype=np.float32)
    c["ones"] = np.ones((128, 128), np.float32)
    j = np.arange(128)
    c["tri"] = (j[:, None] > j[None, :]).astype(np.float32)
    q = np.arange(512)
    m4 = np.zeros((128, 4, 512), np.float32)
    for jj in range(4):
        m4[:, jj, :] = ((128 * jj + j)[:, None] < q[None, :])
    c["maskS4"] = m4
    t = np.arange(S)
    tb = t // 64
    jb = np.arange(32)
    causal = jb[None, :] <= tb[:, None]
    forced = (jb[None, :] == 0) | (causal & (jb[None, :] > tb[:, None] - 2))
    cnf = (causal & ~forced).astype(np.float32)
    add = np.where(forced, 1e4, np.where(causal, 0.0, -1e4)).astype(np.float32)
    c["cnf"] = np.ascontiguousarray(cnf.reshape(16, 128, 32).transpose(1, 0, 2))
    c["add"] = np.ascontiguousarray(add.reshape(16, 128, 32).transpose(1, 0, 2))
    ci = np.arange(127)[:, None]
    sj = np.arange(32)[None, :]
    ov = ((ci * 16 <= sj * 64 + 63) & (ci * 16 + 31 >= sj * 64)).astype(np.float32)
    c["overlap"] = np.concatenate([ov, np.zeros((1, 32), np.float32)], 0)
    es = np.zeros((32, 16, 128), np.float32)
    for kb in range(16):
        es[2 * kb, kb, :64] = 1
        es[2 * kb + 1, kb, 64:] = 1
    c["esel"] = es.astype(ml_dtypes.bfloat16)
    sg = np.zeros((24, 24, 128), np.float32)
    for i in range(24):
        sg[i, i, :] = 1
    c["selg"] = sg.astype(ml_dtypes.bfloat16)
    se = np.zeros((8, 8, 128), np.float32)
    for i in range(8):
        se[i, i, :] = 1
    c["sele"] = se
    table = rel_bias.astype(np.float32)
    cend = np.arange(127) * 16 + 31
    dist_c = t[None, :] - cend[:, None]
    bc = np.full((8, 128, S), NEG, np.float32)
    bk = _rel_bucket(dist_c)
    for h in range(8):
        bc[h, :127] = np.where(dist_c >= 0, table[bk, h], NEG)
    c["biasC"] = bc
    k = np.arange(128)
    bn = np.zeros((8, 128, 5, 512), np.float32)
    offs_n = [0, -128, -256, -384, 128]
    for ti, o in enumerate(offs_n):
        dist = o + q[None, :] - k[:, None]
        bkt = _rel_bucket(dist)
        for h in range(8):
            bn[h, :, ti, :] = np.where(dist >= 0, table[bkt, h], NEG)
    c["biasN"] = bn
    bw = np.zeros((8, 128, 8, 512), np.float32)
    offs_w = [0, -128, -256, -384, 128, 256, 384, 512]
    for ti, o in enumerate(offs_w):
        dist = o + q[None, :] - k[:, None]
        bkt = _rel_bucket(dist)
        for h in range(8):
            bw[h, :, ti, :] = np.where((dist >= 0) & (dist < 512), table[bkt, h], NEG)
    c["biasW"] = bw
    c["b31"] = np.ascontiguousarray(np.broadcast_to(table[31][None, :], (128, 8))).astype(np.float32)
    return c


CONST_SHAPES = {
    "ident": ([128, 128], F32), "ones": ([128, 128], F32), "tri": ([128, 128], F32),
    "maskS4": ([128, 4, 512], F32), "cnf": ([128, 16, 32], F32), "add": ([128, 16, 32], F32),
    "overlap": ([128, 32], F32), "esel": ([32, 16, 128], BF16), "selg": ([24, 24, 128], BF16),
    "sele": ([8, 8, 128], F32), "biasC": ([8, 128, S], F32), "biasN": ([8, 128, 5, 512], F32),
    "biasW": ([8, 128, 8, 512], F32), "b31": ([128, 8], F32),
}

WEIGHT_SHAPES = {
    "w_ada": [L, D, 6 * D], "w_in": [L, D, NIN], "w_conv_out": [L, 1024, D], "w_sb_out": [L, 1024, D],
    "w_nsa_out": [L, 1024, D], "w_o": [L, D, D], "nsa_cmp_wk": [L, 4096, 128], "nsa_cmp_wv": [L, 4096, 128],
    "ffn_w1": [2, D, DFF], "ffn_w3": [2, D, DFF], "ffn_w2": [2, DFF, D],
    "moe_router": [2, D, NE], "moe_w1": [2, NE, D, DFE], "moe_w3": [2, NE, D, DFE], "moe_w2": [2, NE, DFE, D],
}


def build(T=2048, nlayers=L, debug=(), stop=None):
    nc = bass.Bass("TRN2", target_bir_lowering=False)
    NTC = T // 512
    dr = {}

    def din(name, shape, dt=F32):
        dr[name] = nc.dram_tensor(name, list(shape), dt, kind="ExternalInput").ap()
        return dr[name]

    def scr(name, shape, dt):
        if name in debug:
            dr[name] = nc.dram_tensor(name, list(shape), dt, kind="ExternalOutput").ap()
        else:
            dr[name] = nc.dram_tensor(name, list(shape), dt).ap()
        return dr[name]

    x_d = din("x", [T, D])
    cT_d = din("cT", [128, 16])
    vecs_d = din("vecs", [L, 128, NV])
    rb_d = din("rbias", [2, 128, NE])
    class _Lazy(dict):
        def __missing__(self, k):
            if "@" in k:
                nm, li = k.split("@")
                ap = din(nm + "_" + li, WEIGHT_SHAPES[nm][1:])
                self[k] = ap
                return ap
            if k.startswith("k_"):
                shp, dt = CONST_SHAPES[k[2:]]
                return din(k, shp, dt)
            raise KeyError(k)
    dr = _Lazy(dr)
    out_d = nc.dram_tensor("out", [T, D], F32, kind="ExternalOutput").ap()

    XT = scr("XT", [16, 128, T], F32)
    VT = scr("VT", [8, 128, T], F32)
    YLN = scr("YLN", [8, 128, T], BF16)
    QSB = scr("QSB", [8, 128, T], BF16)
    KSB = scr("KSB", [8, 128, T], BF16)
    VSB = scr("VSB", [T, 1024], BF16)
    QN = scr("QN", [8, 128, T], BF16)
    KC = scr("KC", [2, 128, T], BF16)
    VC = scr("VC", [2, 128, T], BF16)
    KS = scr("KS", [2, 128, T], BF16)
    VS = scr("VS", [T, 256], BF16)
    KW = scr("KW", [2, 128, T], BF16)
    VW = scr("VW", [T, 256], BF16)
    GN = scr("GN", [24, T], F32)
    GM = scr("GM", [48, 128, T], BF16)
    OSB = scr("OSB", [8, 128, T], BF16)
    ONSA = scr("ONSA", [8, 128, T], BF16)
    SELD = scr("SELD", [2, 32, T], BF16)

    with ExitStack() as st:
        P = Prog(nc, st)
        ec = st.enter_context

        uid = [0]

        def sb(name, shape, dt, stack=None):
            uid[0] += 1
            return (stack or st).enter_context(nc.sbuf_tensor(f"{name}_{uid[0]}", list(shape), dt))
        cur = {}

        ident = sb("ident", [128, 128], F32)
        ones = sb("ones", [128, 128], F32)
        onesb = sb("onesb", [128, 128], BF16)
        epst = sb("epst", [128, 1], F32)
        onet = sb("onet", [128, 1], F32)
        VEC = sb("VEC", [128, NV], F32)
        MOD = sb("MOD", [128, 96], F32)
        AMIX = sb("AMIX", [128, 16], F32)
        AFFN = sb("AFFN", [128, 16], F32)
        cact = sb("cact", [128, 16], BF16)
        WB = Rot([sb(f"WB{i}", [128, 16, 512], BF16) for i in range(2)], "WB")
        ps = [ec(nc.psum_tensor(f"ps{i}", [128, 512], F32)) for i in range(8)]
        bps = [Buf(f"ps{i}") for i in range(8)]
        b_const = Buf("const")
        b_vec = Buf("vec")
        b_mod = Buf("mod")
        b_HT = [Buf(f"HT{i}") for i in range(NTC)]
        bd = {k: Buf(k) for k in ["XT", "VT", "YLN", "QSB", "KSB", "VSB", "QN", "KC", "VC", "KS", "VS", "KW", "VW",
                                  "GN", "GM", "OSB", "ONSA", "SELD", "out"]}
        pctr = [0]

        def bank():
            j = pctr[0] % 8
            pctr[0] += 1
            return ps[j], bps[j]

        def wview(ap2d, kc):
            return ap2d.rearrange("(kc p) n -> p kc n", p=128)

        def load_w(src, KC, ncols, rot=None):
            t, b = (rot or WB).next()
            P.dma("pool", t[:, 0:KC, 0:ncols], wview(src, KC), W=[b])
            return t, b

        def end_phase():
            P.drain_dmas()

        with nc.Block():
            P.dma("sp", ident[:, :], dr["k_ident"], W=[b_const])
            P.dma("sp", ones[:, :], dr["k_ones"], W=[b_const])
            P.I("dve", "tensor_copy", onesb[:, :], ones[:, :], R=[b_const], W=[b_const])
            P.I("dve", "memset", epst[:, :], EPS, W=[b_const])
            P.I("dve", "memset", onet[:, :], 1.0, W=[b_const])
            with ExitStack() as ph:
                xin = Rot([sb(f"xin{i}", [128, D], F32, ph) for i in range(2)], "xin")
                xo = Rot([sb(f"xo{i}", [128, 512], F32, ph) for i in range(3)], "xo")
                ctmp = sb("ctmp", [128, 16], F32, ph)
                b_ct = Buf("ct")
                P.dma("sp", ctmp[:, :], cT_d, W=[b_ct])
                P.I("act", "activation", cact[:, :], ctmp[:, :], AF.Silu, R=[b_ct], W=[b_mod])
                k = 0
                for tb in range(T // 128):
                    xt, bx = xin.next()
                    P.dma("sp", xt[:, :], x_d[tb * 128:(tb + 1) * 128, :], W=[bx])
                    for g4 in range(4):
                        pt, bp = bank()
                        for j in range(4):
                            dc = g4 * 4 + j
                            P.I("pe", "transpose", pt[:, j * 128:(j + 1) * 128], xt[:, dc * 128:(dc + 1) * 128], ident[:, :],
                                R=[bx, b_const], W=[bp], sig=(j == 3))
                        o, bo = xo.next()
                        if k % 2 == 0:
                            P.I("act", "activation", o[:, :], pt[:, :], AF.Copy, R=[bp], W=[bo])
                        else:
                            P.I("dve", "tensor_copy", o[:, :], pt[:, :], R=[bp], W=[bo])
                        k += 1
                        P.dma("sp", XT[g4 * 4:(g4 + 1) * 4, :, tb * 128:(tb + 1) * 128].rearrange("c p t -> p c t"),
                              o[:, :].rearrange("p (c t) -> p c t", c=4), R=[bo], A=[bd["XT"]])
                end_phase()

        def adaln(l):
            with nc.Block():
                P.dma("sp", VEC[:, :], vecs_d[l], W=[b_vec])
                pm, bpm = bank()
                blocks = [(nb) for nb in range(24)]
                nxt = load_w(dr[f"w_ada@{l}"][:, 0:512], 16, 512)
                for nb in blocks:
                    wt, bw = nxt
                    if nb + 1 < 24:
                        nxt = load_w(dr[f"w_ada@{l}"][:, (nb + 1) * 512:(nb + 2) * 512], 16, 512)
                    for mc in range(4):
                        j = nb * 4 + mc
                        for kc in range(16):
                            P.I("pe", "matmul", pm[:, j:j + 1], wt[:, kc, mc * 128:(mc + 1) * 128], cact[:, kc:kc + 1],
                                start=(kc == 0), stop=(kc == 15), R=[bw, b_mod], W=[bpm], sig=(kc == 15 and mc == 3))
                P.I("dve", "tensor_tensor", MOD[:, :], pm[:, 0:96], VEC[:, V_BADA:V_BADA + 96], ALU.add,
                    R=[bpm, b_vec], W=[b_mod])
                P.I("dve", "scalar_tensor_tensor", AMIX[:, :], MOD[:, 16:32], 1.0, VEC[:, V_GMIX:V_GMIX + 16], ALU.add, ALU.mult,
                    R=[b_mod, b_vec], W=[b_mod])
                P.I("dve", "scalar_tensor_tensor", AFFN[:, :], MOD[:, 64:80], 1.0, VEC[:, V_GFFN:V_GFFN + 16], ALU.add, ALU.mult,
                    R=[b_mod, b_vec], W=[b_mod])
                end_phase()

        def norm_phase(ph, t0, nt, Avec, sh_col, h32cb=None):
            for b_ in b_HT:
                b_.w = dict(b_.w)
            xt = sb("n_xt", [128, 16, 512], F32, ph)
            bxt = Buf("n_xt")
            sq = Rot([sb(f"n_sq{i}", [128, 512], F32, ph) for i in range(2)], "n_sq")
            tm = Rot([sb(f"n_tm{i}", [128, 512], F32, ph) for i in range(2)], "n_tm")
            rs = sb("n_rs", [128, 512], F32, ph)
            brs = Buf("n_rs")
            XTv = XT.rearrange("c p t -> p c t")
            for tcn in range(nt // 512):
                c0 = t0 + tcn * 512
                P.dma("sp", xt[:, :, :], XTv[:, :, c0:c0 + 512], R=[bd["XT"]], W=[bxt])
                pt, bp = bank()
                for kc in range(16):
                    s, bs = sq.next()
                    P.I("act", "activation", s[:, :], xt[:, kc, :], AF.Square, R=[bxt], W=[bs])
                    P.I("pe", "matmul", pt[:, :], ones[:, :], s[:, :], start=(kc == 0), stop=(kc == 15),
                        R=[bs, b_const], W=[bp])
                P.I("act", "activation", rs[:, :], pt[:, :], AF.Ln, bias=epst[:, 0:1], scale=1.0 / D, R=[bp, b_const], W=[brs])
                P.I("act", "activation", rs[:, :], rs[:, :], AF.Exp, scale=-0.5, R=[brs], W=[brs])
                for kc in range(16):
                    t_, bt = tm.next()
                    P.I("dve", "tensor_tensor", t_[:, :], xt[:, kc, :], rs[:, :], ALU.mult, R=[bxt, brs], W=[bt])
                    P.I("act", "activation", cur["HT"][:, kc, tcn * 512:(tcn + 1) * 512], t_[:, :], AF.Identity,
                        bias=MOD[:, sh_col + kc:sh_col + kc + 1], scale=Avec[:, kc:kc + 1], R=[bt, b_mod], A=[b_HT[tcn]])
                    if h32cb is not None:
                        h32cb(tcn, kc, t_, bt)
                if h32cb is not None:
                    h32cb(tcn, None, None, None)

        def proj_fm(wsrc, KC, col0, ncols, rhs_fn, ntc, epi):
            blks = []
            c = 0
            while c < ncols:
                n = min(512, ncols - c)
                blks.append((c, n))
                c += n
            nxt = load_w(wsrc[:, col0 + blks[0][0]:col0 + blks[0][0] + blks[0][1]], KC, blks[0][1])
            for bi, (c, n) in enumerate(blks):
                wt, bw = nxt
                if bi + 1 < len(blks):
                    c2, n2 = blks[bi + 1]
                    nxt = load_w(wsrc[:, col0 + c2:col0 + c2 + n2], KC, n2)
                m = 0
                while m < n:
                    msz = min(128, n - m)
                    for tcn in range(ntc):
                        pt, bp = bank()
                        for kc in range(KC):
                            ra, rb = rhs_fn(kc, tcn)
                            P.I("pe", "matmul", pt[0:msz, :], wt[:, kc, m:m + msz], ra, start=(kc == 0), stop=(kc == KC - 1),
                                R=[bw] + rb, W=[bp], sig=(kc == KC - 1))
                        epi(c + m, msz, tcn, pt, bp)
                    m += msz

        def ht_rhs(kc, tcn):
            return cur["HT"][:, kc, tcn * 512:(tcn + 1) * 512], [b_HT[tcn]]

        def inproj_phase(l):
            w_in = dr[f"w_in@{l}"]
            with nc.Block(), ExitStack() as ph:
                ot = Rot([sb(f"ip_o{i}", [128, 512], BF16, ph) for i in range(3)], "ip_o")
                of = Rot([sb(f"ip_f{i}", [128, 512], F32, ph) for i in range(3)], "ip_f")
                ek = [0]

                def store_copy(dst, bdst):
                    def epi(c, msz, tcn, pt, bp):
                        o, bo = ot.next()
                        if ek[0] % 2 == 0:
                            P.I("act", "activation", o[0:msz, :], pt[0:msz, :], AF.Copy, R=[bp], W=[bo])
                        else:
                            P.I("dve", "tensor_copy", o[0:msz, :], pt[0:msz, :], R=[bp], W=[bo])
                        ek[0] += 1
                        P.dma("sp", dst[c // 128, :, tcn * 512:(tcn + 1) * 512], o[0:msz, :], R=[bo], A=[bdst])
                    return epi

                def store_sig(dst_fn, bdst, odt_rot):
                    def epi(c, msz, tcn, pt, bp):
                        o, bo = odt_rot.next()
                        P.I("act", "activation", o[0:msz, :], pt[0:msz, :], AF.Sigmoid, R=[bp], W=[bo])
                        P.dma("sp", dst_fn(c, msz, tcn), o[0:msz, :], R=[bo], A=[bdst])
                    return epi

                def store_rms(dst, bdst, gcol):
                    def epi(c, msz, tcn, pt, bp):
                        s, bs = of.next()
                        P.I("act", "activation", s[:, :], pt[:, :], AF.Square, R=[bp], W=[bs])
                        p2, bp2 = bank()
                        P.I("pe", "matmul", p2[:, :], ones[:, :], s[:, :], start=True, stop=True, R=[bs, b_const], W=[bp2])
                        r, br = of.next()
                        P.I("act", "activation", r[:, :], p2[:, :], AF.Ln, bias=epst[:, 0:1], scale=1.0 / 128, R=[bp2, b_const], W=[br])
                        P.I("act", "activation", r[:, :], r[:, :], AF.Exp, scale=-0.5, R=[br], W=[br])
                        o, bo = ot.next()
                        P.I("dve", "scalar_tensor_tensor", o[:, :], pt[:, :], VEC[:, gcol:gcol + 1], r[:, :], ALU.mult, ALU.mult,
                            R=[bp, br, b_vec], W=[bo])
                        P.dma("sp", dst[c // 128, :, tcn * 512:(tcn + 1) * 512], o[:, :], R=[bo], A=[bdst])
                    return epi

                U = sb("cv_U", [128, 30 + T], F32, ph)
                bU = Buf("U")
                ACC = sb("cv_acc", [128, T], F32, ph)
                bACC = Buf("acc")
                SUM = sb("cv_sum", [128, T], F32, ph)
                SQS = sb("cv_sqs", [128, T], F32, ph)
                bSUM = Buf("sum")
                P.I("pool", "memset", U[:, 0:30], 0.0, A=[bU])
                for g4 in range(2):
                    wa, bwa = load_w(w_in[:, g4 * 512:(g4 + 1) * 512], 16, 512)
                    wg, bwg = load_w(w_in[:, 1024 + g4 * 512:1024 + (g4 + 1) * 512], 16, 512)
                    for j in range(4):
                        cc = g4 * 4 + j
                        for tcn in range(NTC):
                            pa, bpa = bank()
                            pg, bpg = bank()
                            for kc in range(16):
                                P.I("pe", "matmul", pg[:, :], wg[:, kc, j * 128:(j + 1) * 128], cur["HT"][:, kc, tcn * 512:(tcn + 1) * 512],
                                    start=(kc == 0), stop=(kc == 15), R=[bwg, b_HT[tcn]], W=[bpg], sig=(kc == 15))
                            for kc in range(16):
                                P.I("pe", "matmul", pa[:, :], wa[:, kc, j * 128:(j + 1) * 128], cur["HT"][:, kc, tcn * 512:(tcn + 1) * 512],
                                    start=(kc == 0), stop=(kc == 15), R=[bwa, b_HT[tcn]], W=[bpa], sig=(kc == 15))
                            s, bs = of.next()
                            P.I("act", "activation", s[:, :], pg[:, :], AF.Sigmoid, R=[bpg], W=[bs])
                            P.I("dve", "tensor_tensor", U[:, 30 + tcn * 512:30 + (tcn + 1) * 512], pa[:, :], s[:, :], ALU.mult,
                                R=[bpa, bs], A=[bU])
                        cw = V_CW + cc * 31
                        P.I("dve", "tensor_scalar", ACC[:, :], U[:, 30:30 + T], VEC[:, cw + 30:cw + 31], VEC[:, V_CB + cc:V_CB + cc + 1],
                            ALU.mult, ALU.add, R=[bU, b_vec], W=[bACC])
                        for k in range(30):
                            P.I("dve", "scalar_tensor_tensor", ACC[:, :], U[:, k:k + T], VEC[:, cw + k:cw + k + 1], ACC[:, :],
                                ALU.mult, ALU.add, R=[bU, b_vec], W=[bACC])
                        for tcn in range(NTC):
                            sl = slice(tcn * 512, (tcn + 1) * 512)
                            s, bs = of.next()
                            P.I("act", "activation", s[:, :], ACC[:, sl], AF.Square, R=[bACC], W=[bs])
                            p1, bp1 = bank()
                            P.I("pe", "matmul", p1[:, :], ones[:, :], ACC[:, sl], start=True, stop=True, R=[bACC, b_const], W=[bp1])
                            p2, bp2 = bank()
                            P.I("pe", "matmul", p2[:, :], ones[:, :], s[:, :], start=True, stop=True, R=[bs, b_const], W=[bp2])
                            if cc == 0:
                                P.I("dve", "tensor_copy", SUM[:, sl], p1[:, :], R=[bp1], W=[bSUM])
                                P.I("dve", "tensor_copy", SQS[:, sl], p2[:, :], R=[bp2], W=[bSUM])
                            else:
                                P.I("dve", "tensor_tensor", SUM[:, sl], SUM[:, sl], p1[:, :], ALU.add, R=[bp1], W=[bSUM])
                                P.I("dve", "tensor_tensor", SQS[:, sl], SQS[:, sl], p2[:, :], ALU.add, R=[bp2], W=[bSUM])
                        P.dma("sp", VT[cc], ACC[:, :], R=[bACC], A=[bd["VT"]])
                mean = sb("cv_mean", [128, 512], F32, ph)
                m2 = sb("cv_m2", [128, 512], F32, ph)
                bm = Buf("mean")
                bm2 = Buf("m2")
                for tcn in range(NTC):
                    sl = slice(tcn * 512, (tcn + 1) * 512)
                    P.I("dve", "tensor_scalar", mean[:, :], SUM[:, sl], 1.0 / 1024, None, ALU.mult, R=[bSUM], W=[bm])
                    P.I("dve", "tensor_tensor", m2[:, :], mean[:, :], mean[:, :], ALU.mult, R=[bm], W=[bm2])
                    P.I("dve", "scalar_tensor_tensor", m2[:, :], SQS[:, sl], 1.0 / 1024, m2[:, :], ALU.mult, ALU.subtract,
                        R=[bSUM], W=[bm2])
                    P.I("act", "activation", m2[:, :], m2[:, :], AF.Ln, bias=epst[:, 0:1], scale=1.0, R=[b_const], W=[bm2])
                    P.I("act", "activation", m2[:, :], m2[:, :], AF.Exp, scale=-0.5, W=[bm2])
                    for cc in range(8):
                        v, bv = of.next()
                        P.dma("sp", v[:, :], VT[cc][:, sl], R=[bd["VT"]], W=[bv])
                        P.I("dve", "tensor_tensor", v[:, :], v[:, :], mean[:, :], ALU.subtract, R=[bm], W=[bv])
                        P.I("pool", "tensor_tensor", v[:, :], v[:, :], m2[:, :], ALU.mult, R=[bm2], W=[bv])
                        o, bo = ot.next()
                        P.I("act", "activation", o[:, :], v[:, :], AF.Silu, bias=VEC[:, V_LNB + cc:V_LNB + cc + 1],
                            scale=VEC[:, V_LNG + cc:V_LNG + cc + 1], R=[bv, b_vec], W=[bo])
                        P.dma("sp", YLN[cc][:, sl], o[:, :], R=[bo], A=[bd["YLN"]])

                proj_fm(w_in, 16, C_SBQ, 1024, ht_rhs, NTC, store_copy(QSB, bd["QSB"]))
                proj_fm(w_in, 16, C_SBK, 1024, ht_rhs, NTC, store_copy(KSB, bd["KSB"]))
                proj_fm(w_in, 16, C_KC, 256, ht_rhs, NTC, store_copy(KC, bd["KC"]))
                proj_fm(w_in, 16, C_VC, 256, ht_rhs, NTC, store_copy(VC, bd["VC"]))
                proj_fm(w_in, 16, C_NQ, 1024, ht_rhs, NTC, store_rms(QN, bd["QN"], V_QG))
                proj_fm(w_in, 16, C_KS, 256, ht_rhs, NTC, store_rms(KS, bd["KS"], V_KSG))
                proj_fm(w_in, 16, C_KW, 256, ht_rhs, NTC, store_rms(KW, bd["KW"], V_KWG))
                proj_fm(w_in, 16, C_NG, 24, ht_rhs, NTC,
                        store_sig(lambda c, msz, tcn: GN[0:24, tcn * 512:(tcn + 1) * 512], bd["GN"], of))
                proj_fm(w_in, 16, C_MG, 6144, ht_rhs, NTC,
                        store_sig(lambda c, msz, tcn: GM[c // 128, :, tcn * 512:(tcn + 1) * 512], bd["GM"], ot))
                for (c0, ncols, dst, bdst) in ((C_SBV, 1024, VSB, bd["VSB"]), (C_VS, 256, VS, bd["VS"]), (C_VW, 256, VW, bd["VW"])):
                    for cb in range(0, ncols, 512):
                        n = min(512, ncols - cb)
                        wt, bw = load_w(w_in[:, c0 + cb:c0 + cb + n], 16, n)
                        for tb in range(T // 128):
                            pt, bp = bank()
                            for kc in range(16):
                                P.I("pe", "matmul", pt[:, 0:n], cur["HT"][:, kc, tb * 128:(tb + 1) * 128], wt[:, kc, 0:n],
                                    start=(kc == 0), stop=(kc == 15), R=[bw, b_HT[tb // 4]], W=[bp], sig=(kc == 15))
                            o, bo = ot.next()
                            if tb % 2 == 0:
                                P.I("act", "activation", o[:, 0:n], pt[:, 0:n], AF.Copy, R=[bp], W=[bo])
                            else:
                                P.I("dve", "tensor_copy", o[:, 0:n], pt[:, 0:n], R=[bp], W=[bo])
                            P.dma("sp", dst[tb * 128:(tb + 1) * 128, cb:cb + n], o[:, 0:n], R=[bo], A=[bdst])
                end_phase()

        actr = [0]
        octr = [0]

        def bankA():
            j = actr[0] % 6
            actr[0] += 1
            return ps[j], bps[j]

        def bankO():
            j = 6 + octr[0] % 2
            octr[0] += 1
            return ps[j], bps[j]

        b_c2 = Buf("const2")
        SCALE = 128 ** -0.5

        def sb_phase():
            with nc.Block(), ExitStack() as ph:
                tri = sb("sb_tri", [128, 128], F32, ph)
                m4 = sb("sb_m4", [128, 4, 512], F32, ph)
                P.dma("sp", tri[:, :], dr["k_tri"], W=[b_c2])
                P.dma("sp", m4[:, :, :], dr["k_maskS4"], A=[b_c2])
                QT = Rot([sb(f"sb_q{i}", [128, T], BF16, ph) for i in range(2)], "sbq")
                KT = Rot([sb(f"sb_k{i}", [128, T], BF16, ph) for i in range(2)], "sbk")
                VH = Rot([sb(f"sb_v{i}", [128, T // 128, 128], BF16, ph) for i in range(2)], "sbv")
                E = Rot([sb(f"sb_e{i}", [128, 512], F32, ph) for i in range(2)], "sbe")
                SP = Rot([sb(f"sb_sp{i}", [128, 512], F32, ph) for i in range(2)], "sbsp")
                NL = Rot([sb(f"sb_n{i}", [128, 512], F32, ph) for i in range(2)], "sbn")
                TM = Rot([sb(f"sb_t{i}", [128, 512], F32, ph) for i in range(2)], "sbt")
                AT = Rot([sb(f"sb_a{i}", [128, 512], BF16, ph) for i in range(3)], "sba")
                OO = Rot([sb(f"sb_o{i}", [128, 512], BF16, ph) for i in range(2)], "sbo")
                LACC = sb("sb_lacc", [128, 512], F32, ph)
                bL = Buf("lacc")
                for h in range(8):
                    q, bq = QT.next()
                    P.dma("sp", q[:, :], QSB[h], R=[bd["QSB"]], W=[bq])
                    k_, bk = KT.next()
                    P.dma("sp", k_[:, :], KSB[h], R=[bd["KSB"]], W=[bk])
                    v, bv = VH.next()
                    P.dma("sp", v[:, :, :], VSB[:, h * 128:(h + 1) * 128].rearrange("(b p) d -> p b d", p=128),
                          R=[bd["VSB"]], W=[bv])
                    for qc in range(NTC):
                        cs = slice(qc * 512, (qc + 1) * 512)
                        po, bpo = bankO()
                        kbs = list(range(4 * qc + 3, -1, -1))
                        for si, kb in enumerate(kbs):
                            first = si == 0
                            last = si == len(kbs) - 1
                            j = kb - 4 * qc
                            pz, bpz = bankA()
                            P.I("pe", "matmul", pz[:, :], k_[:, kb * 128:(kb + 1) * 128], q[:, cs], start=True, stop=True,
                                R=[bk, bq], W=[bpz])
                            e, be = E.next()
                            P.I("act", "activation", e[:, :], pz[:, :], AF.Exp, scale=-SCALE, R=[bpz], W=[be])
                            sp_, bsp = SP.next()
                            P.I("act", "activation", sp_[:, :], e[:, :], AF.Ln, bias=onet[:, 0:1], scale=1.0, R=[be, b_const], W=[bsp])
                            n, bn = NL.next()
                            P.I("dve", "scalar_tensor_tensor", n[:, :], pz[:, :], SCALE, sp_[:, :], ALU.mult, ALU.add,
                                R=[bpz, bsp], W=[bn])
                            if j >= 0:
                                P.I("dve", "tensor_tensor", n[:, :], n[:, :], m4[:, j, :], ALU.mult, R=[b_c2], W=[bn])
                            pn, bpn = bankA()
                            P.I("pe", "matmul", pn[:, :], tri[:, :], n[:, :], start=True, stop=first, R=[bn, b_c2], W=[bpn])
                            if not first:
                                P.I("pe", "matmul", pn[:, :], ones[:, :], LACC[:, :], start=False, stop=True, R=[bL, b_const], W=[bpn])
                            t_, bt = TM.next()
                            P.I("dve", "tensor_tensor", t_[:, :], pn[:, :], sp_[:, :], ALU.add, R=[bpn, bsp], W=[bt])
                            a, ba = AT.next()
                            P.I("act", "activation", a[:, :], t_[:, :], AF.Exp, scale=-1.0, R=[bt], W=[ba])
                            if j >= 0:
                                P.I("pool", "tensor_tensor", a[:, :], a[:, :], m4[:, j, :], ALU.mult, R=[b_c2], W=[ba])
                            P.I("pe", "matmul", po[:, :], v[:, kb, :], a[:, :], start=first, stop=last, R=[bv, ba], W=[bpo])
                            if not last:
                                if first:
                                    P.I("pool", "tensor_copy", LACC[:, :], n[:, :], R=[bn], W=[bL])
                                else:
                                    P.I("pool", "tensor_tensor", LACC[:, :], LACC[:, :], n[:, :], ALU.add, R=[bn], W=[bL])
                        o, bo = OO.next()
                        P.I("act", "activation", o[:, :], po[:, :], AF.Copy, R=[bpo], W=[bo])
                        P.dma("sp", OSB[h][:, cs], o[:, :], R=[bo], A=[bd["OSB"]])
                end_phase()

        def nsa_phase(l):
            with nc.Block(), ExitStack() as ph:
                cnf = sb("ns_cnf", [128, 16, 32], F32, ph)
                addc = sb("ns_add", [128, 16, 32], F32, ph)
                ovl = sb("ns_ovl", [128, 32], F32, ph)
                esel = sb("ns_esel", [32, 16, 128], BF16, ph)
                selg = sb("ns_selg", [24, 24, 128], BF16, ph)
                b31 = sb("ns_b31", [128, 8], F32, ph)
                P.dma("sp", cnf[:, :, :], dr["k_cnf"], W=[b_c2])
                P.dma("sp", addc[:, :, :], dr["k_add"], A=[b_c2])
                P.dma("sp", ovl[:, :], dr["k_overlap"], A=[b_c2])
                P.dma("sp", esel[:, :, :], dr["k_esel"], A=[b_c2])
                P.dma("sp", b31[:, :], dr["k_b31"], A=[b_c2])
                bsg = Buf("selg")
                P.dma("sp", selg[:, :, :], dr["k_selg"], W=[bsg])
                GT = sb("ns_GT", [24, T], BF16, ph)
                bGT = Buf("GT")
                for c_ in range(0, T, 1024):
                    P.dma("pool", GT[:, c_:c_ + 1024], GN[:, c_:c_ + 1024], R=[bd["GN"]], A=[bGT])
                OACC = [sb(f"ns_oacc{r}", [128, T], F32, ph) for r in range(4)]
                bOA = [Buf(f"oacc{r}") for r in range(4)]
                SELT = sb("ns_selT", [32, T], BF16, ph)
                bSEL = Buf("selT")
                kcT = sb("ns_kcT", [128, T], BF16, ph)
                vcT = sb("ns_vcT", [128, T], BF16, ph)
                bkc, bvc = Buf("kcT"), Buf("vcT")
                WC = Rot([sb(f"ns_wc{i}", [128, 32, 128], BF16, ph) for i in range(1)], "wc")
                TL = Rot([sb(f"ns_tl{i}", [128, 128], BF16, ph) for i in range(3)], "tl")
                OF = Rot([sb(f"ns_of{i}", [128, 512], F32, ph) for i in range(4)], "nof")
                EB = Rot([sb(f"ns_eb{i}", [128, 512], BF16, ph) for i in range(3)], "neb")
                QH = Rot([sb(f"ns_qh{i}", [128, 512], BF16, ph) for i in range(3)], "nqh")
                BC = Rot([sb(f"ns_bc{i}", [128, 512], F32, ph) for i in range(2)], "nbc")
                SC = Rot([sb(f"ns_sc{i}", [128, 32], F32, ph) for i in range(2)], "nsc")
                P4 = sb("ns_p4", [128, 4, 512], F32, ph)
                bP4 = [Buf(f"p4{r}") for r in range(4)]
                ssq = sb("ns_ssq", [128, 1], F32, ph)
                bss = Buf("ssq")
                kn = sb("ns_kn", [128, 128], F32, ph)
                bkn = Buf("kn")
                kcbT = sb("ns_kcbT", [128, 128], BF16, ph)
                bkcb = Buf("kcbT")
                vcb = sb("ns_vcb", [128, 128], F32, ph)
                bvcb = Buf("vcb")
                sc2 = sb("ns_sc2", [128, 32], F32, ph)
                m1 = sb("ns_m1", [128, 8], F32, ph)
                m2 = sb("ns_m2", [128, 8], F32, ph)
                sel = sb("ns_sel", [128, 32], F32, ph)
                btk = Buf("topk")
                kT = sb("ns_kT", [128, T], BF16, ph)
                vv = sb("ns_vv", [128, T // 128, 128], BF16, ph)
                qhf = sb("ns_qhf", [128, T], BF16, ph)
                btile = sb("ns_bt", [128, 8, 512], F32, ph)
                bkT, bvv, bqhf, bbt = Buf("kT"), Buf("vv"), Buf("qhf"), Buf("bt")
                P.I("dve", "memset", kn[:, :], 0.0, W=[bkn])

                def gate_bc(br, h, cs):
                    pG, bpG = bankA()
                    P.I("pe", "matmul", pG[:, :], selg[:, br * 8 + h, :], GT[0:24, cs], start=True, stop=True,
                        R=[bsg, bGT], W=[bpG])
                    gs, bgs = OF.next()
                    P.I("act", "activation", gs[:, :], pG[:, :], AF.Copy, R=[bpG], W=[bgs])
                    return gs, bgs

                def attn_branch(g, KTd, bKTd, Vd, bVd, bias_key, ntile, tmap, br, use_sel, kb_fn):
                    P.dma("sp", kT[:, :], KTd[g], R=[bKTd], W=[bkT])
                    P.dma("sp", vv[:, :, :], Vd[:, g * 128:(g + 1) * 128].rearrange("(b p) d -> p b d", p=128), R=[bVd], W=[bvv])
                    for r in range(4):
                        h = 4 * g + r
                        P.dma("sp", btile[:, 0:ntile, :], dr[bias_key][h], W=[bbt])
                        P.dma("sp", qhf[:, :], QN[h], R=[bd["QN"]], W=[bqhf])
                        for qc in range(NTC):
                            cs = slice(qc * 512, (qc + 1) * 512)
                            kbs = kb_fn(qc)
                            pO, bpO = bankO()
                            pSm, bpSm = bankO()
                            for si, kb in enumerate(kbs):
                                first = si == 0
                                last = si == len(kbs) - 1
                                ti = tmap.get(512 * qc - 128 * kb)
                                pS, bpS = bankA()
                                P.I("pe", "matmul", pS[:, :], kT[:, kb * 128:(kb + 1) * 128], qhf[:, cs], start=True, stop=True,
                                    R=[bkT, bqhf], W=[bpS])
                                e, be = EB.next()
                                if ti is None:
                                    P.I("act", "activation", e[:, :], pS[:, :], AF.Exp, bias=b31[:, h:h + 1], scale=SCALE,
                                        R=[bpS, b_c2], W=[be])
                                else:
                                    lg, blg = OF.next()
                                    P.I("dve", "scalar_tensor_tensor", lg[:, :], pS[:, :], SCALE, btile[:, ti, :], ALU.mult, ALU.add,
                                        R=[bpS, bbt], W=[blg])
                                    P.I("act", "activation", e[:, :], lg[:, :], AF.Exp, R=[blg], W=[be])
                                if use_sel:
                                    pM, bpM = bankA()
                                    P.I("pe", "matmul", pM[:, :], esel[:, kb, :], SELT[0:32, cs], start=True, stop=True,
                                        R=[b_c2, bSEL], W=[bpM])
                                    P.I("dve", "tensor_tensor", e[:, :], e[:, :], pM[:, :], ALU.mult, R=[bpM], W=[be])
                                P.I("pe", "matmul", pSm[:, :], onesb[:, :], e[:, :], start=first, stop=last, R=[be, b_const], W=[bpSm])
                                P.I("pe", "matmul", pO[:, :], vv[:, kb, :], e[:, :], start=first, stop=last, R=[be, bvv], W=[bpO])
                            rv, brv = OF.next()
                            P.I("dve", "tensor_scalar", rv[:, :], pSm[:, :], 1e-20, None, ALU.max, R=[bpSm], W=[brv])
                            P.I("dve", "reciprocal", rv[:, :], rv[:, :], W=[brv])
                            gs, bgs = gate_bc(br, h, cs)
                            P.I("pool", "tensor_tensor", rv[:, :], rv[:, :], gs[:, :], ALU.mult, R=[bgs], W=[brv])
                            t_, bt = OF.next()
                            P.I("dve", "tensor_tensor", t_[:, :], pO[:, :], rv[:, :], ALU.mult, R=[bpO, brv], W=[bt])
                            P.I("pool", "tensor_tensor", OACC[r][:, cs], OACC[r][:, cs], t_[:, :], ALU.add, R=[bt], W=[bOA[r]])

                tmap_n = {0: 0, -128: 1, -256: 2, -384: 3, 128: 4}
                tmap_w = {0: 0, -128: 1, -256: 2, -384: 3, 128: 4, 256: 5, 384: 6, 512: 7}
                for g in range(2):
                    P.dma("sp", kcT[:, :], KC[g], R=[bd["KC"]], W=[bkc])
                    P.dma("sp", vcT[:, :], VC[g], R=[bd["VC"]], W=[bvc])
                    for (srcT, bsrc, wname, posc, is_k) in ((kcT, bkc, "nsa_cmp_wk", V_PK, True), (vcT, bvc, "nsa_cmp_wv", V_PV, False)):
                        w, bw = WC.next()
                        P.dma("pool", w[:, :, :], dr[f"{wname}@{l}"].rearrange("(l d) o -> d l o", d=128), W=[bw])
                        pc, bpc = bankA()
                        sv = srcT[:, :].rearrange("p (c s) -> p c s", s=16)
                        for l_ in range(32):
                            a_, r_ = divmod(l_, 16)
                            tl, btl = TL.next()
                            P.I("dve", "tensor_scalar", tl[:, 0:127], sv[:, a_:a_ + 127, r_], VEC[:, posc + l_:posc + l_ + 1], None, ALU.add,
                                R=[bsrc, b_vec], W=[btl])
                            P.I("pe", "matmul", pc[0:127, 0:128], tl[:, 0:127], w[:, l_, :], start=(l_ == 0), stop=(l_ == 31),
                                R=[btl, bw], W=[bpc])
                        if is_k:
                            jk, bj = OF.next()
                            P.I("act", "activation", jk[0:127, 0:128], pc[0:127, 0:128], AF.Square, accum_out=ssq[0:127, 0:1],
                                R=[bpc], W=[bj, bss])
                            P.I("act", "activation", ssq[0:127, :], ssq[0:127, :], AF.Ln, bias=epst[0:127, 0:1], scale=1.0 / 128,
                                R=[b_const], W=[bss])
                            P.I("act", "activation", ssq[0:127, :], ssq[0:127, :], AF.Exp, scale=-0.5, W=[bss])
                            P.I("dve", "tensor_scalar", kn[0:127, :], pc[0:127, 0:128], ssq[0:127, 0:1], None, ALU.mult,
                                R=[bpc, bss], W=[bkn])
                            pT, bpT = bankA()
                            P.I("pe", "transpose", pT[:, 0:128], kn[:, :], ident[:, :], R=[bkn, b_const], W=[bpT])
                            P.I("dve", "tensor_scalar", kcbT[:, :], pT[:, 0:128], VEC[:, V_KCG:V_KCG + 1], None, ALU.mult,
                                R=[bpT, b_vec], W=[bkcb])
                        else:
                            P.I("act", "activation", vcb[0:127, :], pc[0:127, 0:128], AF.Copy, R=[bpc], W=[bvcb])
                    for qc in range(NTC):
                        cs = slice(qc * 512, (qc + 1) * 512)
                        for r in range(4):
                            h = 4 * g + r
                            qh, bqh = QH.next()
                            P.dma("sp", qh[:, :], QN[h][:, cs], R=[bd["QN"]], W=[bqh])
                            bC, bbC = BC.next()
                            P.dma("sp", bC[:, :], dr["k_biasC"][h][:, cs], W=[bbC])
                            pS, bpS = bankA()
                            P.I("pe", "matmul", pS[0:127, :], kcbT[:, 0:127], qh[:, :], start=True, stop=True, R=[bkcb, bqh], W=[bpS])
                            lg, blg = OF.next()
                            P.I("dve", "scalar_tensor_tensor", lg[0:127, :], pS[0:127, :], SCALE, bC[0:127, :], ALU.mult, ALU.add,
                                R=[bpS, bbC], W=[blg])
                            P.I("act", "activation", P4[0:127, r, :], lg[0:127, :], AF.Exp, R=[blg], W=[bP4[r]])
                            pSm, bpSm = bankA()
                            P.I("pe", "matmul", pSm[:, :], ones[0:127, :], P4[0:127, r, :], start=True, stop=True,
                                R=[bP4[r], b_const], W=[bpSm])
                            rv, brv = OF.next()
                            P.I("dve", "tensor_scalar", rv[:, :], pSm[:, :], 1e-20, None, ALU.max, R=[bpSm], W=[brv])
                            P.I("dve", "reciprocal", rv[:, :], rv[:, :], W=[brv])
                            P.I("pool", "tensor_tensor", P4[0:127, r, :], P4[0:127, r, :], rv[0:127, :], ALU.mult, R=[brv], W=[bP4[r]])
                            pO, bpO = bankA()
                            P.I("pe", "matmul", pO[:, :], vcb[0:127, :], P4[0:127, r, :], start=True, stop=True,
                                R=[bvcb, bP4[r]], W=[bpO])
                            gs, bgs = gate_bc(0, h, cs)
                            P.I("dve", "tensor_tensor", OACC[r][:, cs], pO[:, :], gs[:, :], ALU.mult, R=[bpO, bgs], W=[bOA[r]])
                        for qs in range(4):
                            qb = qc * 4 + qs
                            pI, bpI = bankA()
                            for r in range(4):
                                P.I("pe", "matmul", pI[:, 0:32], P4[0:127, r, qs * 128:(qs + 1) * 128], ovl[0:127, :],
                                    start=(r == 0), stop=(r == 3), R=[bP4[r], b_c2], W=[bpI])
                            sc_, bsc = SC.next()
                            P.I("dve", "tensor_tensor", sc_[:, :], pI[:, 0:32], cnf[:, qb, :], ALU.mult, R=[bpI, b_c2], W=[bsc])
                            P.I("dve", "tensor_tensor", sc_[:, :], sc_[:, :], addc[:, qb, :], ALU.add, R=[b_c2], W=[bsc])
                            P.I("dve", "max", m1[:, :], sc_[:, :], R=[bsc], W=[btk])
                            P.I("dve", "match_replace", sc2[:, :], m1[:, :], sc_[:, :], -1e30, R=[bsc], W=[btk])
                            P.I("dve", "max", m2[:, :], sc2[:, :], W=[btk])
                            P.I("dve", "tensor_scalar", sel[:, :], sc_[:, :], m2[:, 7:8], None, ALU.is_ge, R=[bsc], W=[btk])
                            P.I("dve", "scalar_tensor_tensor", sel[:, :], sc_[:, :], -5000.0, sel[:, :], ALU.is_gt, ALU.mult,
                                R=[bsc], W=[btk])
                            pT, bpT = bankA()
                            P.I("pe", "transpose", pT[0:32, 0:128], sel[:, 0:32], ident[:, :], R=[btk, b_const], W=[bpT])
                            P.I("act", "activation", SELT[0:32, qb * 128:(qb + 1) * 128], pT[0:32, 0:128], AF.Copy,
                                R=[bpT], W=[bSEL])
                    if "SELD" in debug:
                        P.dma("sp", SELD[g], SELT[0:32, :], R=[bSEL], A=[bd["SELD"]])
                    attn_branch(g, KS, bd["KS"], VS, bd["VS"], "k_biasN", 5, tmap_n, 1, True,
                                lambda qc: list(range(4 * qc + 3, -1, -1)))
                    attn_branch(g, KW, bd["KW"], VW, bd["VW"], "k_biasW", 8, tmap_w, 2, False,
                                lambda qc: list(range(4 * qc + 3, max(0, 4 * qc - 4) - 1, -1)))
                    for r in range(4):
                        h = 4 * g + r
                        for qc in range(NTC):
                            cs = slice(qc * 512, (qc + 1) * 512)
                            o, bo = EB.next()
                            P.I("act", "activation", o[:, :], OACC[r][:, cs], AF.Copy, R=[bOA[r]], W=[bo])
                            P.dma("sp", ONSA[h][:, cs], o[:, :], R=[bo], A=[bd["ONSA"]])
                end_phase()

        def make_resid(ph, ga_col, t0):
            XR = Rot([sb(f"rs_x{i}", [128, 512], F32, ph) for i in range(3)], "rsx")

            def epi(c, msz, tcn, pt, bp):
                dc = c // 128
                cols = slice(t0 + tcn * 512, t0 + (tcn + 1) * 512)
                xr, bx = XR.next()
                P.dma("sp", xr[:, :], XT[dc][:, cols], R=[bd["XT"]], W=[bx])
                P.I("dve", "scalar_tensor_tensor", xr[:, :], pt[:, :], MOD[:, ga_col + dc:ga_col + dc + 1], xr[:, :], ALU.mult, ALU.add,
                    R=[bp, b_mod], W=[bx])
                P.dma("sp", XT[dc][:, cols], xr[:, :], R=[bx], A=[bd["XT"]])
            return epi

        def out_phase(l):
            TS = 1024
            for sc_ in range(T // TS):
                t0 = sc_ * TS
                with nc.Block(), ExitStack() as ph:
                    IN = []
                    bIN = []
                    for bi, (src, bsrc) in enumerate(((YLN, bd["YLN"]), (OSB, bd["OSB"]), (ONSA, bd["ONSA"]))):
                        t_ = sb(f"op_in{bi}", [128, 8, TS], BF16, ph)
                        b_ = Buf(f"op_in{bi}")
                        P.dma("sp", t_[:, :, :], src.rearrange("c p t -> p c t")[:, :, t0:t0 + TS], R=[bsrc], W=[b_])
                        IN.append(t_)
                        bIN.append(b_)
                    MT = sb("op_mt", [128, 16, TS], BF16, ph)
                    bMT = [Buf(f"mt{i}") for i in range(TS // 512)]
                    WO = Rot([sb(f"op_w{i}", [128, 8, 512], BF16, ph) for i in range(3)], "opw")
                    GMT = Rot([sb(f"op_g{i}", [128, 512], BF16, ph) for i in range(3)], "opg")
                    ACC = Rot([sb(f"op_acc{i}", [128, 512], F32, ph) for i in range(2)], "opacc")
                    T2 = Rot([sb(f"op_t2{i}", [128, 512], F32, ph) for i in range(2)], "opt2")
                    names = ("w_conv_out", "w_sb_out", "w_nsa_out")
                    for dcg in range(4):
                        wts = [load_w(dr[f"{nm}@{l}"][:, dcg * 512:(dcg + 1) * 512], 8, 512, rot=WO) for nm in names]
                        for dci in range(4):
                            dc = dcg * 4 + dci
                            for tcn in range(TS // 512):
                                ts_ = slice(tcn * 512, (tcn + 1) * 512)
                                acc, bacc = ACC.next()
                                for bi in range(3):
                                    wt, bw = wts[bi]
                                    pY, bpY = bankA()
                                    for cc in range(8):
                                        P.I("pe", "matmul", pY[:, :], wt[:, cc, dci * 128:(dci + 1) * 128], IN[bi][:, cc, ts_],
                                            start=(cc == 0), stop=(cc == 7), R=[bw, bIN[bi]], W=[bpY], sig=(cc == 7))
                                    gm, bgm = GMT.next()
                                    P.dma("sp", gm[:, :], GM[bi * 16 + dc][:, t0 + tcn * 512:t0 + (tcn + 1) * 512], R=[bd["GM"]], W=[bgm])
                                    if bi == 0:
                                        P.I("dve", "tensor_tensor", acc[:, :], pY[:, :], gm[:, :], ALU.mult, R=[bpY, bgm], W=[bacc])
                                    else:
                                        t2, bt2 = T2.next()
                                        P.I("dve", "tensor_tensor", t2[:, :], pY[:, :], gm[:, :], ALU.mult, R=[bpY, bgm], W=[bt2])
                                        P.I("pool", "tensor_tensor", acc[:, :], acc[:, :], t2[:, :], ALU.add, R=[bt2], W=[bacc])
                                P.I("act", "activation", MT[:, dc, ts_], acc[:, :], AF.Copy, R=[bacc], A=[bMT[tcn]])
                    proj_fm(dr[f"w_o@{l}"], 16, 0, D, lambda kc, tcn: (MT[:, kc, tcn * 512:(tcn + 1) * 512], [bMT[tcn]]),
                            TS // 512, make_resid(ph, 32, t0))
                    end_phase()

        def ffn_phase(l):
            i = l // 2
            moe = (l % 2 == 1)
            TS = 1024
            for sc_ in range(T // TS):
                t0 = sc_ * TS
                with ExitStack() as fs:
                    HT2 = sb("f_HT", [128, 16, TS], BF16, fs)
                    cur["HT"] = HT2
                    CT = sb("f_CT", [8, TS], F32, fs) if moe else None
                    bCT = Buf("CT")
                    with nc.Block(), ExitStack() as ph:
                        cb = None
                        if moe:
                            H32 = sb("f_h32", [128, 16, 512], F32, ph)
                            bH32 = Buf("h32")
                            wr = sb("f_wr", [128, 16, NE], F32, ph)
                            rbt = sb("f_rb", [128, NE], F32, ph)
                            bwr = Buf("wr")
                            P.dma("sp", wr[:, :, :], dr[f"moe_router@{i}"].rearrange("(kc p) e -> p kc e", p=128), W=[bwr])
                            P.dma("sp", rbt[:, :], rb_d[i], A=[bwr])
                            lgt = sb("f_lgt", [128, NE], F32, ph)
                            mx = sb("f_mx", [128, 8], F32, ph)
                            sm = sb("f_sm", [128, 4], F32, ph)
                            ex = sb("f_ex", [128, NE], F32, ph)
                            cmb = sb("f_cmb", [128, NE], F32, ph)
                            brt = Buf("router")

                            def cb(tcn, kc, t_, bt):
                                if kc is not None:
                                    P.I("act", "activation", H32[:, kc, :], t_[:, :], AF.Identity, bias=MOD[:, 48 + kc:48 + kc + 1],
                                        scale=AFFN[:, kc:kc + 1], R=[bt, b_mod], A=[bH32])
                                    return
                                for qs in range(4):
                                    pR, bpR = bankA()
                                    for kc2 in range(16):
                                        P.I("pe", "matmul", pR[:, 0:NE], H32[:, kc2, qs * 128:(qs + 1) * 128], wr[:, kc2, :],
                                            start=(kc2 == 0), stop=(kc2 == 15), R=[bH32, bwr], W=[bpR], sig=(kc2 == 15))
                                    P.I("dve", "tensor_tensor", lgt[:, :], pR[:, 0:NE], rbt[:, :], ALU.add, R=[bpR, bwr], W=[brt])
                                    P.I("dve", "max", mx[:, :], lgt[:, :], W=[brt])
                                    P.I("dve", "tensor_scalar", sm[:, 0:1], mx[:, 0:1], -1.0, None, ALU.mult, W=[brt])
                                    P.I("act", "activation", ex[:, :], lgt[:, :], AF.Exp, bias=sm[:, 0:1], scale=1.0, W=[brt])
                                    P.I("act", "activation", sm[:, 1:2], mx[:, 1:2], AF.Exp, bias=sm[:, 0:1], scale=1.0, W=[brt])
                                    P.I("dve", "tensor_scalar", sm[:, 1:2], sm[:, 1:2], 1.0, None, ALU.add, W=[brt])
                                    P.I("dve", "reciprocal", sm[:, 2:3], sm[:, 1:2], W=[brt])
                                    P.I("dve", "tensor_scalar", cmb[:, :], lgt[:, :], mx[:, 1:2], None, ALU.is_ge, W=[brt])
                                    P.I("dve", "scalar_tensor_tensor", cmb[:, :], ex[:, :], sm[:, 2:3], cmb[:, :], ALU.mult, ALU.mult, W=[brt])
                                    pT, bpT = bankA()
                                    P.I("pe", "transpose", pT[0:NE, 0:128], cmb[:, 0:NE], ident[:, :], R=[brt, b_const], W=[bpT])
                                    c0 = tcn * 512 + qs * 128
                                    P.I("act", "activation", CT[0:NE, c0:c0 + 128], pT[0:NE, 0:128], AF.Copy, R=[bpT], A=[bCT])
                                bH32.w = dict(bH32.w)
                        norm_phase(ph, t0, TS, AFFN, 48, h32cb=cb)
                        end_phase()
                    with nc.Block(), ExitStack() as ph:
                        nfc = (DFE if moe else DFF) // 128
                        AT = sb("f_AT", [128, nfc, TS], BF16, ph)
                        bAT = [Buf(f"AT{t}") for t in range(TS // 512)]
                        SL = Rot([sb(f"f_s{k}", [128, 512], F32, ph) for k in range(3)], "fs")
                        W2 = Rot([sb(f"f_w2{k}", [128, nfc, 128], BF16, ph) for k in range(2)], "fw2")
                        resid = make_resid(ph, 80, t0)
                        sele = None
                        if moe:
                            sele = sb("f_sele", [8, 8, 128], F32, ph)
                            P.dma("sp", sele[:, :, :], dr["k_sele"], W=[b_c2])
                            cbs = [sb(f"f_cb{t}", [128, 512], F32, ph) for t in range(TS // 512)]
                            bcbs = [Buf("cb") for t in range(TS // 512)]
                        for e_ in range(NE if moe else 1):
                            if moe:
                                w1d, w3d, w2d = dr[f"moe_w1@{i}"][e_], dr[f"moe_w3@{i}"][e_], dr[f"moe_w2@{i}"][e_]
                                dff = DFE
                                for tcn in range(TS // 512):
                                    pC, bpC = bankA()
                                    P.I("pe", "matmul", pC[:, :], sele[:, e_, :], CT[0:NE, tcn * 512:(tcn + 1) * 512], start=True, stop=True,
                                        R=[b_c2, bCT], W=[bpC])
                                    P.I("act", "activation", cbs[tcn][:, :], pC[:, :], AF.Copy, R=[bpC], W=[bcbs[tcn]])
                            else:
                                w1d, w3d, w2d = dr[f"ffn_w1@{i}"], dr[f"ffn_w3@{i}"], dr[f"ffn_w2@{i}"]
                                dff = DFF
                            c = 0
                            while c < dff:
                                n = min(512, dff - c)
                                w1, bw1 = load_w(w1d[:, c:c + n], 16, n)
                                w3, bw3 = load_w(w3d[:, c:c + n], 16, n)
                                for m in range(0, n, 128):
                                    fc = (c + m) // 128
                                    for tcn in range(TS // 512):
                                        ts_ = slice(tcn * 512, (tcn + 1) * 512)
                                        p1, bp1 = bankA()
                                        p3, bp3 = bankA()
                                        for kc in range(16):
                                            P.I("pe", "matmul", p1[:, :], w1[:, kc, m:m + 128], HT2[:, kc, ts_], start=(kc == 0), stop=(kc == 15),
                                                R=[bw1, b_HT[tcn]], W=[bp1], sig=(kc == 15))
                                        for kc in range(16):
                                            P.I("pe", "matmul", p3[:, :], w3[:, kc, m:m + 128], HT2[:, kc, ts_], start=(kc == 0), stop=(kc == 15),
                                                R=[bw3, b_HT[tcn]], W=[bp3], sig=(kc == 15))
                                        s_, bs_ = SL.next()
                                        P.I("act", "activation", s_[:, :], p1[:, :], AF.Silu, R=[bp1], W=[bs_])
                                        if moe:
                                            P.I("dve", "tensor_tensor", s_[:, :], s_[:, :], p3[:, :], ALU.mult, R=[bp3], W=[bs_])
                                            P.I("pool", "tensor_tensor", AT[:, fc, ts_], s_[:, :], cbs[tcn][:, :], ALU.mult,
                                                R=[bs_, bcbs[tcn]], A=[bAT[tcn]])
                                        else:
                                            P.I("dve", "tensor_tensor", AT[:, fc, ts_], s_[:, :], p3[:, :], ALU.mult, R=[bs_, bp3], A=[bAT[tcn]])
                                c += n
                            nf = dff // 128
                            for dc in range(16):
                                w2, bw2 = W2.next()
                                P.dma("pool", w2[:, 0:nf, :], w2d[:, dc * 128:(dc + 1) * 128].rearrange("(kc p) n -> p kc n", p=128), W=[bw2])
                                for tcn in range(TS // 512):
                                    pY, bpY = bankA()
                                    for fc in range(nf):
                                        P.I("pe", "matmul", pY[:, :], w2[:, fc, :], AT[:, fc, tcn * 512:(tcn + 1) * 512],
                                            start=(fc == 0), stop=(fc == nf - 1), R=[bw2, bAT[tcn]], W=[bpY], sig=(fc == nf - 1))
                                    resid(dc * 128, 128, tcn, pY, bpY)
                            for b_ in bAT:
                                b_.w = dict(b_.w)
                        end_phase()

        for l in range(nlayers):
            if stop == "p0":
                break
            adaln(l)
            if stop == "adaln":
                break
            with ExitStack() as hs:
                cur["HT"] = sb("HTm", [128, 16, T], BF16, hs)
                with nc.Block(), ExitStack() as ph:
                    norm_phase(ph, 0, T, AMIX, 0)
                    end_phase()
                if stop == "norm":
                    break
                inproj_phase(l)
            if stop == "inproj":
                break
            sb_phase()
            if stop == "sb":
                break
            nsa_phase(l)
            if stop == "nsa":
                break
            out_phase(l)
            if stop == "out":
                break
            ffn_phase(l)

        with nc.Block(), ExitStack() as ph:
            fin = Rot([sb(f"f_in{i}", [128, 16, 128], F32, ph) for i in range(2)], "f_in")
            fo = Rot([sb(f"f_o{i}", [128, D], F32, ph) for i in range(2)], "f_o")
            XTv = XT.rearrange("c p t -> p c t")
            for tb in range(T // 128):
                xi, bxi = fin.next()
                P.dma("sp", xi[:, :, :], XTv[:, :, tb * 128:(tb + 1) * 128], R=[bd["XT"]], W=[bxi])
                o, bo = fo.next()
                for g4 in range(4):
                    pt, bp = bank()
                    for j in range(4):
                        P.I("pe", "transpose", pt[:, j * 128:(j + 1) * 128], xi[:, g4 * 4 + j, :], ident[:, :],
                            R=[bxi, b_const], W=[bp], sig=(j == 3))
                    if g4 % 2 == 0:
                        P.I("act", "activation", o[:, g4 * 512:(g4 + 1) * 512], pt[:, :], AF.Copy, R=[bp], A=[bo])
                    else:
                        P.I("dve", "tensor_copy", o[:, g4 * 512:(g4 + 1) * 512], pt[:, :], R=[bp], A=[bo])
                P.dma("sp", out_d[tb * 128:(tb + 1) * 128, :], o[:, :], R=[bo], A=[bd["out"]])
            P.drain_dmas()
        print("instructions", P.n_ins, "waits", P.n_wait)
    return nc, dr


def _vecs(inp):
    v = np.zeros((L, 128, NV), np.float32)

    def pm(a, n):
        return np.ascontiguousarray(a.reshape(n, 128).T)
    for l in range(L):
        v[l, :, V_BADA:V_BADA + 96] = pm(inp["b_ada"][l], 96)
        v[l, :, V_GMIX:V_GMIX + 16] = pm(inp["g_mix"][l], 16)
        v[l, :, V_GFFN:V_GFFN + 16] = pm(inp["g_ffn"][l], 16)
        v[l, :, V_CB:V_CB + 8] = pm(inp["conv_b"][l], 8)
        v[l, :, V_LNG:V_LNG + 8] = pm(inp["conv_ln_g"][l], 8)
        v[l, :, V_LNB:V_LNB + 8] = pm(inp["conv_ln_b"][l], 8)
        cw = inp["conv_w"][l]
        v[l, :, V_CW:V_CW + 248] = cw.reshape(31, 8, 128).transpose(2, 1, 0).reshape(128, 248)
        v[l, :, V_QG] = inp["nsa_q_g"][l]
        v[l, :, V_KCG] = inp["nsa_kc_g"][l]
        v[l, :, V_KSG] = inp["nsa_ks_g"][l]
        v[l, :, V_KWG] = inp["nsa_kw_g"][l]
        v[l, :, V_PK:V_PK + 32] = inp["nsa_cmp_pos_k"][l].T
        v[l, :, V_PV:V_PV + 32] = inp["nsa_cmp_pos_v"][l].T
    return v


def prep_shared(inp):
    sh = {}
    for k in WEIGHT_SHAPES:
        if k in inp:
            a = np.asarray(inp[k], dtype=np.float32)
            for li in range(a.shape[0]):
                sh[f"{k}_{li}"] = a[li]
    sh["vecs"] = _vecs(inp)
    sh["rbias"] = np.ascontiguousarray(np.broadcast_to(np.asarray(inp["moe_router_b"], np.float32)[:, None, :], (2, 128, NE)))
    for k, a in _host_consts(np.asarray(inp["rel_bias"], np.float32)).items():
        sh["k_" + k] = np.ascontiguousarray(a)
    return sh


def prep_core(inp, b, T=2048):
    d = {}
    d["x"] = np.ascontiguousarray(np.asarray(inp["x"][b], np.float32)[:T])
    d["cT"] = np.ascontiguousarray(np.asarray(inp["c"][b], np.float32).reshape(16, 128).T)
    return d


N_LAYERS_IMPL = L
STOP_AT = None
N_CORES = 4


def kernel(**inputs):
    T = 2048
    nc, dr = build(T=T, nlayers=N_LAYERS_IMPL, stop=STOP_AT)
    names = set()
    for k in dr:
        ap = dr[k]
        names.add(ap.tensor.name)
    sh = prep_shared(inputs)
    in_maps = []
    for core in range(N_CORES):
        b = core % 4
        m = dict(sh)
        m.update(prep_core(inputs, b, T))
        in_maps.append({k: np.ascontiguousarray(v) for k, v in m.items() if k in names})
    res = run_bass_kernel_spmd(nc, in_maps, core_ids=list(range(N_CORES)))
    out = np.stack([np.asarray(res.results[b]["out"], dtype=np.float32) for b in range(4)], 0)
    return out
```

```python
import math
from contextlib import ExitStack

import numpy as np
import ml_dtypes
import concourse.bass as bass
import concourse.mybir as mybir
from concourse.bass_utils import run_bass_kernel_spmd

F32 = mybir.dt.float32
BF16 = mybir.dt.bfloat16
AF = mybir.ActivationFunctionType
ALU = mybir.AluOpType

L = 4
D = 2048
S = 2048
NIN = 13848
DFF = 5632
NE = 8
DFE = 2816
EPS = 1e-6
NEG = -30000.0
C_GLU, C_SBQ, C_SBK, C_SBV, C_NQ, C_KC, C_VC, C_KS, C_VS, C_KW, C_VW, C_NG, C_MG = (
    0, 2048, 3072, 4096, 5120, 6144, 6400, 6656, 6912, 7168, 7424, 7680, 7704)
V_BADA, V_GMIX, V_GFFN, V_CB, V_LNG, V_LNB, V_CW, V_QG, V_KCG, V_KSG, V_KWG, V_PK, V_PV, NV = (
    0, 96, 112, 128, 136, 144, 152, 400, 401, 402, 403, 404, 436, 468)


class Buf:
    __slots__ = ("name", "w", "r")

    def __init__(self, name=""):
        self.name = name
        self.w = {}
        self.r = {}


class Prog:
    def __init__(self, nc, stack, n_dma_sems=(24, 24)):
        self.nc = nc
        self.engs = {"pe": nc.tensor, "dve": nc.vector, "act": nc.scalar,
                     "pool": nc.gpsimd, "sp": nc.sync}
        self.sems = {}
        self.cnt = {}
        for e in self.engs:
            self.sems[e] = stack.enter_context(nc.semaphore("c_" + e))
            self.cnt[e] = 0
        self.dq = {}
        for q, n in zip(("sp", "pool"), n_dma_sems):
            lst = []
            for i in range(n):
                key = f"d_{q}{i}"
                self.sems[key] = stack.enter_context(nc.semaphore(key))
                self.cnt[key] = 0
                lst.append(key)
            self.dq[q] = [lst, 0]
        self.waited = {e: {} for e in self.engs}
        self.n_wait = 0
        self.n_ins = 0

    def _wait(self, eng, key, val):
        if val <= 0:
            return
        w = self.waited[eng]
        if w.get(key, 0) >= val:
            return
        self.engs[eng].wait_ge(self.sems[key], val)
        w[key] = val
        self.n_wait += 1

    def _deps(self, eng, R, W, A):
        need = {}

        def add(d):
            for k, v in d.items():
                if need.get(k, 0) < v:
                    need[k] = v
        for b in R:
            add(b.w)
        for b in W:
            add(b.w)
            add(b.r)
        for b in A:
            add(b.r)
        for k, v in need.items():
            if eng == "pe" and k == "pe":
                continue
            self._wait(eng, k, v)

    def _commit(self, key, val, R, W, A):
        for b in R:
            if b.r.get(key, 0) < val:
                b.r[key] = val
        for b in W:
            b.w = {key: val}
            b.r = {}
        for b in A:
            if b.w.get(key, 0) < val:
                b.w[key] = val

    def I(self, eng, fn, *args, R=(), W=(), A=(), sig=True, **kw):
        self._deps(eng, R, W, A)
        ins = getattr(self.engs[eng], fn)(*args, **kw)
        self.n_ins += 1
        if sig:
            self.cnt[eng] += 1
            ins.then_inc(self.sems[eng], 1)
            val = self.cnt[eng]
        else:
            val = self.cnt[eng] + 1
        self._commit(eng, val, R, W, A)
        return ins

    def dma(self, q, out, in_, R=(), W=(), A=(), **kw):
        self._deps(q, R, W, A)
        lst, idx = self.dq[q]
        key = lst[idx % len(lst)]
        self.dq[q][1] = idx + 1
        self._wait(q, key, self.cnt[key])
        ins = self.engs[q].dma_start(out=out, in_=in_, **kw)
        self.cnt[key] += 16
        ins.then_inc(self.sems[key], 16)
        self.n_ins += 1
        self._commit(key, self.cnt[key], R, W, A)
        return ins

    def finish(self, eng, bufs):
        for b in bufs:
            for k, v in b.w.items():
                self._wait(eng, k, v)

    def drain_dmas(self):
        for q in self.dq:
            for key in self.dq[q][0]:
                self._wait(q, key, self.cnt[key])


class Rot:
    def __init__(self, tiles, name):
        self.t = tiles
        self.b = [Buf(f"{name}{i}") for i in range(len(tiles))]
        self.i = 0

    def next(self):
        j = self.i % len(self.t)
        self.i += 1
        return self.t[j], self.b[j]


def _rel_bucket(dist):
    n = np.maximum(dist, 0)
    nf = np.maximum(n, 1).astype(np.float32)
    large = 16 + (np.log(nf / np.float32(16)) / np.float32(math.log(128 / 16)) * np.float32(16)).astype(np.int32)
    large = np.minimum(large, 31)
    return np.where(n < 16, n, large)


def _host_consts(rel_bias):
    c = {}
    c["ident"] = np.eye(128, dtype=np.float32)
    c["ones"] = np.ones((128, 128), np.float32)
    j = np.arange(128)
    c["tri"] = (j[:, None] > j[None, :]).astype(np.float32)
    q = np.arange(512)
    m4 = np.zeros((128, 4, 512), np.float32)
    for jj in range(4):
        m4[:, jj, :] = ((128 * jj + j)[:, None] < q[None, :])
    c["maskS4"] = m4
    t = np.arange(S)
    tb = t // 64
    jb = np.arange(32)
    causal = jb[None, :] <= tb[:, None]
    forced = (jb[None, :] == 0) | (causal & (jb[None, :] > tb[:, None] - 2))
    cnf = (causal & ~forced).astype(np.float32)
    add = np.where(forced, 1e4, np.where(causal, 0.0, -1e4)).astype(np.float32)
    c["cnf"] = np.ascontiguousarray(cnf.reshape(16, 128, 32).transpose(1, 0, 2))
    c["add"] = np.ascontiguousarray(add.reshape(16, 128, 32).transpose(1, 0, 2))
    ci = np.arange(127)[:, None]
    sj = np.arange(32)[None, :]
    ov = ((ci * 16 <= sj * 64 + 63) & (ci * 16 + 31 >= sj * 64)).astype(np.float32)
    c["overlap"] = np.concatenate([ov, np.zeros((1, 32), np.float32)], 0)
    es = np.zeros((32, 16, 128), np.float32)
    for kb in range(16):
        es[2 * kb, kb, :64] = 1
        es[2 * kb + 1, kb, 64:] = 1
    c["esel"] = es.astype(ml_dtypes.bfloat16)
    sg = np.zeros((24, 24, 128), np.float32)
    for i in range(24):
        sg[i, i, :] = 1
    c["selg"] = sg.astype(ml_dtypes.bfloat16)
    se = np.zeros((8, 8, 128), np.float32)
    for i in range(8):
        se[i, i, :] = 1
    c["sele"] = se
    table = rel_bias.astype(np.float32)
    cend = np.arange(127) * 16 + 31
    dist_c = t[None, :] - cend[:, None]
    bc = np.full((8, 128, S), NEG, np.float32)
    bk = _rel_bucket(dist_c)
    for h in range(8):
        bc[h, :127] = np.where(dist_c >= 0, table[bk, h], NEG)
    c["biasC"] = bc
    k = np.arange(128)
    bn = np.zeros((8, 128, 5, 512), np.float32)
    offs_n = [0, -128, -256, -384, 128]
    for ti, o in enumerate(offs_n):
        dist = o + q[None, :] - k[:, None]
        bkt = _rel_bucket(dist)
        for h in range(8):
            bn[h, :, ti, :] = np.where(dist >= 0, table[bkt, h], NEG)
    c["biasN"] = bn
    bw = np.zeros((8, 128, 8, 512), np.float32)
    offs_w = [0, -128, -256, -384, 128, 256, 384, 512]
    for ti, o in enumerate(offs_w):
        dist = o + q[None, :] - k[:, None]
        bkt = _rel_bucket(dist)
        for h in range(8):
            bw[h, :, ti, :] = np.where((dist >= 0) & (dist < 512), table[bkt, h], NEG)
    c["biasW"] = bw
    c["b31"] = np.ascontiguousarray(np.broadcast_to(table[31][None, :], (128, 8))).astype(np.float32)
    return c


CONST_SHAPES = {
    "ident": ([128, 128], F32), "ones": ([128, 128], F32), "tri": ([128, 128], F32),
    "maskS4": ([128, 4, 512], F32), "cnf": ([128, 16, 32], F32), "add": ([128, 16, 32], F32),
    "overlap": ([128, 32], F32), "esel": ([32, 16, 128], BF16), "selg": ([24, 24, 128], BF16),
    "sele": ([8, 8, 128], F32), "biasC": ([8, 128, S], F32), "biasN": ([8, 128, 5, 512], F32),
    "biasW": ([8, 128, 8, 512], F32), "b31": ([128, 8], F32),
}

WEIGHT_SHAPES = {
    "w_ada": [L, D, 6 * D], "w_in": [L, D, NIN], "w_conv_out": [L, 1024, D], "w_sb_out": [L, 1024, D],
    "w_nsa_out": [L, 1024, D], "w_o": [L, D, D], "nsa_cmp_wk": [L, 4096, 128], "nsa_cmp_wv": [L, 4096, 128],
    "ffn_w1": [2, D, DFF], "ffn_w3": [2, D, DFF], "ffn_w2": [2, DFF, D],
    "moe_router": [2, D, NE], "moe_w1": [2, NE, D, DFE], "moe_w3": [2, NE, D, DFE], "moe_w2": [2, NE, DFE, D],
}


def build(T=2048, nlayers=L, debug=(), stop=None):
    nc = bass.Bass("TRN2", target_bir_lowering=False)
    NTC = T // 512
    dr = {}

    def din(name, shape, dt=F32):
        dr[name] = nc.dram_tensor(name, list(shape), dt, kind="ExternalInput").ap()
        return dr[name]

    def scr(name, shape, dt):
        if name in debug:
            dr[name] = nc.dram_tensor(name, list(shape), dt, kind="ExternalOutput").ap()
        else:
            dr[name] = nc.dram_tensor(name, list(shape), dt).ap()
        return dr[name]

    x_d = din("x", [T, D])
    cT_d = din("cT", [128, 16])
    vecs_d = din("vecs", [L, 128, NV])
    rb_d = din("rbias", [2, 128, NE])
    class _Lazy(dict):
        def __missing__(self, k):
            if "@" in k:
                nm, li = k.split("@")
                ap = din(nm + "_" + li, WEIGHT_SHAPES[nm][1:])
                self[k] = ap
                return ap
            if k.startswith("k_"):
                shp, dt = CONST_SHAPES[k[2:]]
                return din(k, shp, dt)
            raise KeyError(k)
    dr = _Lazy(dr)
    out_d = nc.dram_tensor("out", [T, D], F32, kind="ExternalOutput").ap()

    XT = scr("XT", [16, 128, T], F32)
    VT = scr("VT", [8, 128, T], F32)
    YLN = scr("YLN", [8, 128, T], BF16)
    QSB = scr("QSB", [8, 128, T], BF16)
    KSB = scr("KSB", [8, 128, T], BF16)
    VSB = scr("VSB", [T, 1024], BF16)
    QN = scr("QN", [8, 128, T], BF16)
    KC = scr("KC", [2, 128, T], BF16)
    VC = scr("VC", [2, 128, T], BF16)
    KS = scr("KS", [2, 128, T], BF16)
    VS = scr("VS", [T, 256], BF16)
    KW = scr("KW", [2, 128, T], BF16)
    VW = scr("VW", [T, 256], BF16)
    GN = scr("GN", [24, T], F32)
    GM = scr("GM", [48, 128, T], BF16)
    OSB = scr("OSB", [8, 128, T], BF16)
    ONSA = scr("ONSA", [8, 128, T], BF16)
    SELD = scr("SELD", [2, 32, T], BF16)

    with ExitStack() as st:
        P = Prog(nc, st)
        ec = st.enter_context

        uid = [0]

        def sb(name, shape, dt, stack=None):
            uid[0] += 1
            return (stack or st).enter_context(nc.sbuf_tensor(f"{name}_{uid[0]}", list(shape), dt))
        cur = {}

        ident = sb("ident", [128, 128], F32)
        ones = sb("ones", [128, 128], F32)
        onesb = sb("onesb", [128, 128], BF16)
        epst = sb("epst", [128, 1], F32)
        onet = sb("onet", [128, 1], F32)
        VEC = sb("VEC", [128, NV], F32)
        MOD = sb("MOD", [128, 96], F32)
        AMIX = sb("AMIX", [128, 16], F32)
        AFFN = sb("AFFN", [128, 16], F32)
        cact = sb("cact", [128, 16], BF16)
        WB = Rot([sb(f"WB{i}", [128, 16, 256], BF16) for i in range(4)], "WB")
        ps = [ec(nc.psum_tensor(f"ps{i}", [128, 512], F32)) for i in range(8)]
        bps = [Buf(f"ps{i}") for i in range(8)]
        b_const = Buf("const")
        b_vec = Buf("vec")
        b_mod = Buf("mod")
        b_HT = [Buf(f"HT{i}") for i in range(NTC)]
        bd = {k: Buf(k) for k in ["XT", "VT", "YLN", "QSB", "KSB", "VSB", "QN", "KC", "VC", "KS", "VS", "KW", "VW",
                                  "GN", "GM", "OSB", "ONSA", "SELD", "out"]}
        pctr = [0]

        def bank():
            j = pctr[0] % 8
            pctr[0] += 1
            return ps[j], bps[j]

        def wview(ap2d, kc):
            return ap2d.rearrange("(kc p) n -> p kc n", p=128)

        def load_w(src, KC, ncols, rot=None):
            t, b = (rot or WB).next()
            P.dma("pool", t[:, 0:KC, 0:ncols], wview(src, KC), W=[b])
            return t, b

        def end_phase():
            P.drain_dmas()

        with nc.Block():
            P.dma("sp", ident[:, :], dr["k_ident"], W=[b_const])
            P.dma("sp", ones[:, :], dr["k_ones"], W=[b_const])
            P.I("dve", "tensor_copy", onesb[:, :], ones[:, :], R=[b_const], W=[b_const])
            P.I("dve", "memset", epst[:, :], EPS, W=[b_const])
            P.I("dve", "memset", onet[:, :], 1.0, W=[b_const])
            with ExitStack() as ph:
                xin = Rot([sb(f"xin{i}", [128, D], F32, ph) for i in range(2)], "xin")
                xo = Rot([sb(f"xo{i}", [128, 512], F32, ph) for i in range(3)], "xo")
                ctmp = sb("ctmp", [128, 16], F32, ph)
                b_ct = Buf("ct")
                P.dma("sp", ctmp[:, :], cT_d, W=[b_ct])
                P.I("act", "activation", cact[:, :], ctmp[:, :], AF.Silu, R=[b_ct], W=[b_mod])
                k = 0
                for tb in range(T // 128):
                    xt, bx = xin.next()
                    P.dma("sp", xt[:, :], x_d[tb * 128:(tb + 1) * 128, :], W=[bx])
                    for g4 in range(4):
                        pt, bp = bank()
                        for j in range(4):
                            dc = g4 * 4 + j
                            P.I("pe", "transpose", pt[:, j * 128:(j + 1) * 128], xt[:, dc * 128:(dc + 1) * 128], ident[:, :],
                                R=[bx, b_const], W=[bp], sig=(j == 3))
                        o, bo = xo.next()
                        if k % 2 == 0:
                            P.I("act", "activation", o[:, :], pt[:, :], AF.Copy, R=[bp], W=[bo])
                        else:
                            P.I("dve", "tensor_copy", o[:, :], pt[:, :], R=[bp], W=[bo])
                        k += 1
                        P.dma("sp", XT[g4 * 4:(g4 + 1) * 4, :, tb * 128:(tb + 1) * 128].rearrange("c p t -> p c t"),
                              o[:, :].rearrange("p (c t) -> p c t", c=4), R=[bo], A=[bd["XT"]])
                end_phase()

        def adaln(l):
            with nc.Block():
                P.dma("sp", VEC[:, :], vecs_d[l], W=[b_vec])
                pm, bpm = bank()
                blocks = [(nb) for nb in range(48)]
                nxt = load_w(dr[f"w_ada@{l}"][:, 0:256], 16, 256)
                for nb in blocks:
                    wt, bw = nxt
                    if nb + 1 < 48:
                        nxt = load_w(dr[f"w_ada@{l}"][:, (nb + 1) * 256:(nb + 2) * 256], 16, 256)
                    for mc in range(2):
                        j = nb * 2 + mc
                        for kc in range(16):
                            P.I("pe", "matmul", pm[:, j:j + 1], wt[:, kc, mc * 128:(mc + 1) * 128], cact[:, kc:kc + 1],
                                start=(kc == 0), stop=(kc == 15), R=[bw, b_mod], W=[bpm], sig=(kc == 15 and mc == 1))
                P.I("dve", "tensor_tensor", MOD[:, :], pm[:, 0:96], VEC[:, V_BADA:V_BADA + 96], ALU.add,
                    R=[bpm, b_vec], W=[b_mod])
                P.I("dve", "scalar_tensor_tensor", AMIX[:, :], MOD[:, 16:32], 1.0, VEC[:, V_GMIX:V_GMIX + 16], ALU.add, ALU.mult,
                    R=[b_mod, b_vec], W=[b_mod])
                P.I("dve", "scalar_tensor_tensor", AFFN[:, :], MOD[:, 64:80], 1.0, VEC[:, V_GFFN:V_GFFN + 16], ALU.add, ALU.mult,
                    R=[b_mod, b_vec], W=[b_mod])
                end_phase()

        def norm_phase(ph, t0, nt, Avec, sh_col, h32cb=None):
            for b_ in b_HT:
                b_.w = dict(b_.w)
            xt = sb("n_xt", [128, 16, 512], F32, ph)
            bxt = Buf("n_xt")
            sq = Rot([sb(f"n_sq{i}", [128, 512], F32, ph) for i in range(2)], "n_sq")
            tm = Rot([sb(f"n_tm{i}", [128, 512], F32, ph) for i in range(2)], "n_tm")
            rs = sb("n_rs", [128, 512], F32, ph)
            brs = Buf("n_rs")
            XTv = XT.rearrange("c p t -> p c t")
            for tcn in range(nt // 512):
                c0 = t0 + tcn * 512
                P.dma("sp", xt[:, :, :], XTv[:, :, c0:c0 + 512], R=[bd["XT"]], W=[bxt])
                pt, bp = bank()
                for kc in range(16):
                    s, bs = sq.next()
                    P.I("act", "activation", s[:, :], xt[:, kc, :], AF.Square, R=[bxt], W=[bs])
                    P.I("pe", "matmul", pt[:, :], ones[:, :], s[:, :], start=(kc == 0), stop=(kc == 15),
                        R=[bs, b_const], W=[bp])
                P.I("act", "activation", rs[:, :], pt[:, :], AF.Ln, bias=epst[:, 0:1], scale=1.0 / D, R=[bp, b_const], W=[brs])
                P.I("act", "activation", rs[:, :], rs[:, :], AF.Exp, scale=-0.5, R=[brs], W=[brs])
                for kc in range(16):
                    t_, bt = tm.next()
                    P.I("dve", "tensor_tensor", t_[:, :], xt[:, kc, :], rs[:, :], ALU.mult, R=[bxt, brs], W=[bt])
                    P.I("act", "activation", cur["HT"][:, kc, tcn * 512:(tcn + 1) * 512], t_[:, :], AF.Identity,
                        bias=MOD[:, sh_col + kc:sh_col + kc + 1], scale=Avec[:, kc:kc + 1], R=[bt, b_mod], A=[b_HT[tcn]])
                    if h32cb is not None:
                        h32cb(tcn, kc, t_, bt)
                if h32cb is not None:
                    h32cb(tcn, None, None, None)

        def proj_fm(wsrc, KC, col0, ncols, rhs_fn, ntc, epi):
            blks = []
            c = 0
            while c < ncols:
                n = min(256, ncols - c)
                blks.append((c, n))
                c += n
            nxt = load_w(wsrc[:, col0 + blks[0][0]:col0 + blks[0][0] + blks[0][1]], KC, blks[0][1])
            for bi, (c, n) in enumerate(blks):
                wt, bw = nxt
                if bi + 1 < len(blks):
                    c2, n2 = blks[bi + 1]
                    nxt = load_w(wsrc[:, col0 + c2:col0 + c2 + n2], KC, n2)
                m = 0
                while m < n:
                    msz = min(128, n - m)
                    for tcn in range(ntc):
                        pt, bp = bank()
                        for kc in range(KC):
                            ra, rb = rhs_fn(kc, tcn)
                            P.I("pe", "matmul", pt[0:msz, :], wt[:, kc, m:m + msz], ra, start=(kc == 0), stop=(kc == KC - 1),
                                R=[bw] + rb, W=[bp], sig=(kc == KC - 1))
                        epi(c + m, msz, tcn, pt, bp)
                    m += msz

        def ht_rhs(kc, tcn):
            return cur["HT"][:, kc, tcn * 512:(tcn + 1) * 512], [b_HT[tcn]]

        def inproj_phase(l):
            w_in = dr[f"w_in@{l}"]
            with nc.Block(), ExitStack() as ph:
                ot = Rot([sb(f"ip_o{i}", [128, 512], BF16, ph) for i in range(3)], "ip_o")
                of = Rot([sb(f"ip_f{i}", [128, 512], F32, ph) for i in range(3)], "ip_f")
                ek = [0]

                def store_copy(dst, bdst):
                    def epi(c, msz, tcn, pt, bp):
                        o, bo = ot.next()
                        if ek[0] % 2 == 0:
                            P.I("act", "activation", o[0:msz, :], pt[0:msz, :], AF.Copy, R=[bp], W=[bo])
                        else:
                            P.I("dve", "tensor_copy", o[0:msz, :], pt[0:msz, :], R=[bp], W=[bo])
                        ek[0] += 1
                        P.dma("sp", dst[c // 128, :, tcn * 512:(tcn + 1) * 512], o[0:msz, :], R=[bo], A=[bdst])
                    return epi

                def store_sig(dst_fn, bdst, odt_rot):
                    def epi(c, msz, tcn, pt, bp):
                        o, bo = odt_rot.next()
                        P.I("act", "activation", o[0:msz, :], pt[0:msz, :], AF.Sigmoid, R=[bp], W=[bo])
                        P.dma("sp", dst_fn(c, msz, tcn), o[0:msz, :], R=[bo], A=[bdst])
                    return epi

                def store_rms(dst, bdst, gcol):
                    def epi(c, msz, tcn, pt, bp):
                        s, bs = of.next()
                        P.I("act", "activation", s[:, :], pt[:, :], AF.Square, R=[bp], W=[bs])
                        p2, bp2 = bank()
                        P.I("pe", "matmul", p2[:, :], ones[:, :], s[:, :], start=True, stop=True, R=[bs, b_const], W=[bp2])
                        r, br = of.next()
                        P.I("act", "activation", r[:, :], p2[:, :], AF.Ln, bias=epst[:, 0:1], scale=1.0 / 128, R=[bp2, b_const], W=[br])
                        P.I("act", "activation", r[:, :], r[:, :], AF.Exp, scale=-0.5, R=[br], W=[br])
                        o, bo = ot.next()
                        P.I("dve", "scalar_tensor_tensor", o[:, :], pt[:, :], VEC[:, gcol:gcol + 1], r[:, :], ALU.mult, ALU.mult,
                            R=[bp, br, b_vec], W=[bo])
                        P.dma("sp", dst[c // 128, :, tcn * 512:(tcn + 1) * 512], o[:, :], R=[bo], A=[bdst])
                    return epi

                U = sb("cv_U", [128, 30 + T], F32, ph)
                bU = Buf("U")
                ACC = sb("cv_acc", [128, T], F32, ph)
                bACC = Buf("acc")
                SUM = sb("cv_sum", [128, T], F32, ph)
                SQS = sb("cv_sqs", [128, T], F32, ph)
                bSUM = Buf("sum")
                P.I("pool", "memset", U[:, 0:30], 0.0, A=[bU])
                for g4 in range(4):
                    wa, bwa = load_w(w_in[:, g4 * 256:(g4 + 1) * 256], 16, 256)
                    wg, bwg = load_w(w_in[:, 1024 + g4 * 256:1024 + (g4 + 1) * 256], 16, 256)
                    for j in range(2):
                        cc = g4 * 2 + j
                        for tcn in range(NTC):
                            pa, bpa = bank()
                            pg, bpg = bank()
                            for kc in range(16):
                                P.I("pe", "matmul", pg[:, :], wg[:, kc, j * 128:(j + 1) * 128], cur["HT"][:, kc, tcn * 512:(tcn + 1) * 512],
                                    start=(kc == 0), stop=(kc == 15), R=[bwg, b_HT[tcn]], W=[bpg], sig=(kc == 15))
                            for kc in range(16):
                                P.I("pe", "matmul", pa[:, :], wa[:, kc, j * 128:(j + 1) * 128], cur["HT"][:, kc, tcn * 512:(tcn + 1) * 512],
                                    start=(kc == 0), stop=(kc == 15), R=[bwa, b_HT[tcn]], W=[bpa], sig=(kc == 15))
                            s, bs = of.next()
                            P.I("act", "activation", s[:, :], pg[:, :], AF.Sigmoid, R=[bpg], W=[bs])
                            P.I("dve", "tensor_tensor", U[:, 30 + tcn * 512:30 + (tcn + 1) * 512], pa[:, :], s[:, :], ALU.mult,
                                R=[bpa, bs], A=[bU])
                        cw = V_CW + cc * 31
                        P.I("dve", "tensor_scalar", ACC[:, :], U[:, 30:30 + T], VEC[:, cw + 30:cw + 31], VEC[:, V_CB + cc:V_CB + cc + 1],
                            ALU.mult, ALU.add, R=[bU, b_vec], W=[bACC])
                        for k in range(30):
                            P.I("dve", "scalar_tensor_tensor", ACC[:, :], U[:, k:k + T], VEC[:, cw + k:cw + k + 1], ACC[:, :],
                                ALU.mult, ALU.add, R=[bU, b_vec], W=[bACC])
                        for tcn in range(NTC):
                            sl = slice(tcn * 512, (tcn + 1) * 512)
                            s, bs = of.next()
                            P.I("act", "activation", s[:, :], ACC[:, sl], AF.Square, R=[bACC], W=[bs])
                            p1, bp1 = bank()
                            P.I("pe", "matmul", p1[:, :], ones[:, :], ACC[:, sl], start=True, stop=True, R=[bACC, b_const], W=[bp1])
                            p2, bp2 = bank()
                            P.I("pe", "matmul", p2[:, :], ones[:, :], s[:, :], start=True, stop=True, R=[bs, b_const], W=[bp2])
                            if cc == 0:
                                P.I("dve", "tensor_copy", SUM[:, sl], p1[:, :], R=[bp1], W=[bSUM])
                                P.I("dve", "tensor_copy", SQS[:, sl], p2[:, :], R=[bp2], W=[bSUM])
                            else:
                                P.I("dve", "tensor_tensor", SUM[:, sl], SUM[:, sl], p1[:, :], ALU.add, R=[bp1], W=[bSUM])
                                P.I("dve", "tensor_tensor", SQS[:, sl], SQS[:, sl], p2[:, :], ALU.add, R=[bp2], W=[bSUM])
                        P.dma("sp", VT[cc], ACC[:, :], R=[bACC], A=[bd["VT"]])
                mean = sb("cv_mean", [128, 512], F32, ph)
                m2 = sb("cv_m2", [128, 512], F32, ph)
                bm = Buf("mean")
                bm2 = Buf("m2")
                for tcn in range(NTC):
                    sl = slice(tcn * 512, (tcn + 1) * 512)
                    P.I("dve", "tensor_scalar", mean[:, :], SUM[:, sl], 1.0 / 1024, None, ALU.mult, R=[bSUM], W=[bm])
                    P.I("dve", "tensor_tensor", m2[:, :], mean[:, :], mean[:, :], ALU.mult, R=[bm], W=[bm2])
                    P.I("dve", "scalar_tensor_tensor", m2[:, :], SQS[:, sl], 1.0 / 1024, m2[:, :], ALU.mult, ALU.subtract,
                        R=[bSUM], W=[bm2])
                    P.I("act", "activation", m2[:, :], m2[:, :], AF.Ln, bias=epst[:, 0:1], scale=1.0, R=[b_const], W=[bm2])
                    P.I("act", "activation", m2[:, :], m2[:, :], AF.Exp, scale=-0.5, W=[bm2])
                    for cc in range(8):
                        v, bv = of.next()
                        P.dma("sp", v[:, :], VT[cc][:, sl], R=[bd["VT"]], W=[bv])
                        P.I("dve", "tensor_tensor", v[:, :], v[:, :], mean[:, :], ALU.subtract, R=[bm], W=[bv])
                        P.I("pool", "tensor_tensor", v[:, :], v[:, :], m2[:, :], ALU.mult, R=[bm2], W=[bv])
                        o, bo = ot.next()
                        P.I("act", "activation", o[:, :], v[:, :], AF.Silu, bias=VEC[:, V_LNB + cc:V_LNB + cc + 1],
                            scale=VEC[:, V_LNG + cc:V_LNG + cc + 1], R=[bv, b_vec], W=[bo])
                        P.dma("sp", YLN[cc][:, sl], o[:, :], R=[bo], A=[bd["YLN"]])

                proj_fm(w_in, 16, C_SBQ, 1024, ht_rhs, NTC, store_copy(QSB, bd["QSB"]))
                proj_fm(w_in, 16, C_SBK, 1024, ht_rhs, NTC, store_copy(KSB, bd["KSB"]))
                proj_fm(w_in, 16, C_KC, 256, ht_rhs, NTC, store_copy(KC, bd["KC"]))
                proj_fm(w_in, 16, C_VC, 256, ht_rhs, NTC, store_copy(VC, bd["VC"]))
                proj_fm(w_in, 16, C_NQ, 1024, ht_rhs, NTC, store_rms(QN, bd["QN"], V_QG))
                proj_fm(w_in, 16, C_KS, 256, ht_rhs, NTC, store_rms(KS, bd["KS"], V_KSG))
                proj_fm(w_in, 16, C_KW, 256, ht_rhs, NTC, store_rms(KW, bd["KW"], V_KWG))
                proj_fm(w_in, 16, C_NG, 24, ht_rhs, NTC,
                        store_sig(lambda c, msz, tcn: GN[0:24, tcn * 512:(tcn + 1) * 512], bd["GN"], of))
                proj_fm(w_in, 16, C_MG, 6144, ht_rhs, NTC,
                        store_sig(lambda c, msz, tcn: GM[c // 128, :, tcn * 512:(tcn + 1) * 512], bd["GM"], ot))
                for (c0, ncols, dst, bdst) in ((C_SBV, 1024, VSB, bd["VSB"]), (C_VS, 256, VS, bd["VS"]), (C_VW, 256, VW, bd["VW"])):
                    for cb in range(0, ncols, 256):
                        n = min(256, ncols - cb)
                        wt, bw = load_w(w_in[:, c0 + cb:c0 + cb + n], 16, n)
                        for tb in range(T // 128):
                            pt, bp = bank()
                            for kc in range(16):
                                P.I("pe", "matmul", pt[:, 0:n], cur["HT"][:, kc, tb * 128:(tb + 1) * 128], wt[:, kc, 0:n],
                                    start=(kc == 0), stop=(kc == 15), R=[bw, b_HT[tb // 4]], W=[bp], sig=(kc == 15))
                            o, bo = ot.next()
                            if tb % 2 == 0:
                                P.I("act", "activation", o[:, 0:n], pt[:, 0:n], AF.Copy, R=[bp], W=[bo])
                            else:
                                P.I("dve", "tensor_copy", o[:, 0:n], pt[:, 0:n], R=[bp], W=[bo])
                            P.dma("sp", dst[tb * 128:(tb + 1) * 128, cb:cb + n], o[:, 0:n], R=[bo], A=[bdst])
                end_phase()

        actr = [0]
        octr = [0]

        def bankA():
            j = actr[0] % 6
            actr[0] += 1
            return ps[j], bps[j]

        def bankO():
            j = 6 + octr[0] % 2
            octr[0] += 1
            return ps[j], bps[j]

        b_c2 = Buf("const2")
        SCALE = 128 ** -0.5

        def sb_phase():
            with nc.Block(), ExitStack() as ph:
                tri = sb("sb_tri", [128, 128], F32, ph)
                m4 = sb("sb_m4", [128, 4, 512], F32, ph)
                P.dma("sp", tri[:, :], dr["k_tri"], W=[b_c2])
                P.dma("sp", m4[:, :, :], dr["k_maskS4"], A=[b_c2])
                QT = Rot([sb(f"sb_q{i}", [128, T], BF16, ph) for i in range(2)], "sbq")
                KT = Rot([sb(f"sb_k{i}", [128, T], BF16, ph) for i in range(2)], "sbk")
                VH = Rot([sb(f"sb_v{i}", [128, T // 128, 128], BF16, ph) for i in range(2)], "sbv")
                E = Rot([sb(f"sb_e{i}", [128, 512], F32, ph) for i in range(2)], "sbe")
                SP = Rot([sb(f"sb_sp{i}", [128, 512], F32, ph) for i in range(2)], "sbsp")
                NL = Rot([sb(f"sb_n{i}", [128, 512], F32, ph) for i in range(2)], "sbn")
                TM = Rot([sb(f"sb_t{i}", [128, 512], F32, ph) for i in range(2)], "sbt")
                AT = Rot([sb(f"sb_a{i}", [128, 512], BF16, ph) for i in range(3)], "sba")
                OO = Rot([sb(f"sb_o{i}", [128, 512], BF16, ph) for i in range(2)], "sbo")
                LACC = sb("sb_lacc", [128, 512], F32, ph)
                bL = Buf("lacc")
                for h in range(8):
                    q, bq = QT.next()
                    P.dma("sp", q[:, :], QSB[h], R=[bd["QSB"]], W=[bq])
                    k_, bk = KT.next()
                    P.dma("sp", k_[:, :], KSB[h], R=[bd["KSB"]], W=[bk])
                    v, bv = VH.next()
                    P.dma("sp", v[:, :, :], VSB[:, h * 128:(h + 1) * 128].rearrange("(b p) d -> p b d", p=128),
                          R=[bd["VSB"]], W=[bv])
                    for qc in range(NTC):
                        cs = slice(qc * 512, (qc + 1) * 512)
                        po, bpo = bankO()
                        kbs = list(range(4 * qc + 3, -1, -1))
                        for si, kb in enumerate(kbs):
                            first = si == 0
                            last = si == len(kbs) - 1
                            j = kb - 4 * qc
                            pz, bpz = bankA()
                            P.I("pe", "matmul", pz[:, :], k_[:, kb * 128:(kb + 1) * 128], q[:, cs], start=True, stop=True,
                                R=[bk, bq], W=[bpz])
                            e, be = E.next()
                            P.I("act", "activation", e[:, :], pz[:, :], AF.Exp, scale=-SCALE, R=[bpz], W=[be])
                            sp_, bsp = SP.next()
                            P.I("act", "activation", sp_[:, :], e[:, :], AF.Ln, bias=onet[:, 0:1], scale=1.0, R=[be, b_const], W=[bsp])
                            n, bn = NL.next()
                            P.I("dve", "scalar_tensor_tensor", n[:, :], pz[:, :], SCALE, sp_[:, :], ALU.mult, ALU.add,
                                R=[bpz, bsp], W=[bn])
                            if j >= 0:
                                P.I("dve", "tensor_tensor", n[:, :], n[:, :], m4[:, j, :], ALU.mult, R=[b_c2], W=[bn])
                            pn, bpn = bankA()
                            P.I("pe", "matmul", pn[:, :], tri[:, :], n[:, :], start=True, stop=first, R=[bn, b_c2], W=[bpn])
                            if not first:
                                P.I("pe", "matmul", pn[:, :], ones[:, :], LACC[:, :], start=False, stop=True, R=[bL, b_const], W=[bpn])
                            t_, bt = TM.next()
                            P.I("dve", "tensor_tensor", t_[:, :], pn[:, :], sp_[:, :], ALU.add, R=[bpn, bsp], W=[bt])
                            a, ba = AT.next()
                            P.I("act", "activation", a[:, :], t_[:, :], AF.Exp, scale=-1.0, R=[bt], W=[ba])
                            if j >= 0:
                                P.I("pool", "tensor_tensor", a[:, :], a[:, :], m4[:, j, :], ALU.mult, R=[b_c2], W=[ba])
                            P.I("pe", "matmul", po[:, :], v[:, kb, :], a[:, :], start=first, stop=last, R=[bv, ba], W=[bpo])
                            if not last:
                                if first:
                                    P.I("pool", "tensor_copy", LACC[:, :], n[:, :], R=[bn], W=[bL])
                                else:
                                    P.I("pool", "tensor_tensor", LACC[:, :], LACC[:, :], n[:, :], ALU.add, R=[bn], W=[bL])
                        o, bo = OO.next()
                        P.I("act", "activation", o[:, :], po[:, :], AF.Copy, R=[bpo], W=[bo])
                        P.dma("sp", OSB[h][:, cs], o[:, :], R=[bo], A=[bd["OSB"]])
                end_phase()

        def nsa_phase(l):
            with nc.Block(), ExitStack() as ph:
                cnf = sb("ns_cnf", [128, 16, 32], F32, ph)
                addc = sb("ns_add", [128, 16, 32], F32, ph)
                ovl = sb("ns_ovl", [128, 32], F32, ph)
                esel = sb("ns_esel", [32, 16, 128], BF16, ph)
                selg = sb("ns_selg", [24, 24, 128], BF16, ph)
                b31 = sb("ns_b31", [128, 8], F32, ph)
                P.dma("sp", cnf[:, :, :], dr["k_cnf"], W=[b_c2])
                P.dma("sp", addc[:, :, :], dr["k_add"], A=[b_c2])
                P.dma("sp", ovl[:, :], dr["k_overlap"], A=[b_c2])
                P.dma("sp", esel[:, :, :], dr["k_esel"], A=[b_c2])
                P.dma("sp", b31[:, :], dr["k_b31"], A=[b_c2])
                bsg = Buf("selg")
                P.dma("sp", selg[:, :, :], dr["k_selg"], W=[bsg])
                GT = sb("ns_GT", [24, T], BF16, ph)
                bGT = Buf("GT")
                for c_ in range(0, T, 1024):
                    P.dma("pool", GT[:, c_:c_ + 1024], GN[:, c_:c_ + 1024], R=[bd["GN"]], A=[bGT])
                OACC = [sb(f"ns_oacc{r}", [128, T], F32, ph) for r in range(4)]
                bOA = [Buf(f"oacc{r}") for r in range(4)]
                SELT = sb("ns_selT", [32, T], BF16, ph)
                bSEL = Buf("selT")
                kcT = sb("ns_kcT", [128, T], BF16, ph)
                vcT = sb("ns_vcT", [128, T], BF16, ph)
                bkc, bvc = Buf("kcT"), Buf("vcT")
                WC = Rot([sb(f"ns_wc{i}", [128, 32, 128], BF16, ph) for i in range(1)], "wc")
                TL = Rot([sb(f"ns_tl{i}", [128, 128], BF16, ph) for i in range(3)], "tl")
                OF = Rot([sb(f"ns_of{i}", [128, 512], F32, ph) for i in range(4)], "nof")
                EB = Rot([sb(f"ns_eb{i}", [128, 512], BF16, ph) for i in range(3)], "neb")
                QH = Rot([sb(f"ns_qh{i}", [128, 512], BF16, ph) for i in range(3)], "nqh")
                BC = Rot([sb(f"ns_bc{i}", [128, 512], F32, ph) for i in range(2)], "nbc")
                SC = Rot([sb(f"ns_sc{i}", [128, 32], F32, ph) for i in range(2)], "nsc")
                P4 = sb("ns_p4", [128, 4, 512], F32, ph)
                bP4 = [Buf(f"p4{r}") for r in range(4)]
                ssq = sb("ns_ssq", [128, 1], F32, ph)
                bss = Buf("ssq")
                kn = sb("ns_kn", [128, 128], F32, ph)
                bkn = Buf("kn")
                kcbT = sb("ns_kcbT", [128, 128], BF16, ph)
                bkcb = Buf("kcbT")
                vcb = sb("ns_vcb", [128, 128], F32, ph)
                bvcb = Buf("vcb")
                sc2 = sb("ns_sc2", [128, 32], F32, ph)
                m1 = sb("ns_m1", [128, 8], F32, ph)
                m2 = sb("ns_m2", [128, 8], F32, ph)
                sel = sb("ns_sel", [128, 32], F32, ph)
                btk = Buf("topk")
                kT = sb("ns_kT", [128, T], BF16, ph)
                vv = sb("ns_vv", [128, T // 128, 128], BF16, ph)
                qhf = sb("ns_qhf", [128, T], BF16, ph)
                btile = sb("ns_bt", [128, 8, 512], F32, ph)
                bkT, bvv, bqhf, bbt = Buf("kT"), Buf("vv"), Buf("qhf"), Buf("bt")
                P.I("dve", "memset", kn[:, :], 0.0, W=[bkn])

                def gate_bc(br, h, cs):
                    pG, bpG = bankA()
                    P.I("pe", "matmul", pG[:, :], selg[:, br * 8 + h, :], GT[0:24, cs], start=True, stop=True,
                        R=[bsg, bGT], W=[bpG])
                    gs, bgs = OF.next()
                    P.I("act", "activation", gs[:, :], pG[:, :], AF.Copy, R=[bpG], W=[bgs])
                    return gs, bgs

                def attn_branch(g, KTd, bKTd, Vd, bVd, bias_key, ntile, tmap, br, use_sel, kb_fn):
                    P.dma("sp", kT[:, :], KTd[g], R=[bKTd], W=[bkT])
                    P.dma("sp", vv[:, :, :], Vd[:, g * 128:(g + 1) * 128].rearrange("(b p) d -> p b d", p=128), R=[bVd], W=[bvv])
                    for r in range(4):
                        h = 4 * g + r
                        P.dma("sp", btile[:, 0:ntile, :], dr[bias_key][h], W=[bbt])
                        P.dma("sp", qhf[:, :], QN[h], R=[bd["QN"]], W=[bqhf])
                        for qc in range(NTC):
                            cs = slice(qc * 512, (qc + 1) * 512)
                            kbs = kb_fn(qc)
                            pO, bpO = bankO()
                            pSm, bpSm = bankO()
                            for si, kb in enumerate(kbs):
                                first = si == 0
                                last = si == len(kbs) - 1
                                ti = tmap.get(512 * qc - 128 * kb)
                                pS, bpS = bankA()
                                P.I("pe", "matmul", pS[:, :], kT[:, kb * 128:(kb + 1) * 128], qhf[:, cs], start=True, stop=True,
                                    R=[bkT, bqhf], W=[bpS])
                                e, be = EB.next()
                                if ti is None:
                                    P.I("act", "activation", e[:, :], pS[:, :], AF.Exp, bias=b31[:, h:h + 1], scale=SCALE,
                                        R=[bpS, b_c2], W=[be])
                                else:
                                    lg, blg = OF.next()
                                    P.I("dve", "scalar_tensor_tensor", lg[:, :], pS[:, :], SCALE, btile[:, ti, :], ALU.mult, ALU.add,
                                        R=[bpS, bbt], W=[blg])
                                    P.I("act", "activation", e[:, :], lg[:, :], AF.Exp, R=[blg], W=[be])
                                if use_sel:
                                    pM, bpM = bankA()
                                    P.I("pe", "matmul", pM[:, :], esel[:, kb, :], SELT[0:32, cs], start=True, stop=True,
                                        R=[b_c2, bSEL], W=[bpM])
                                    P.I("dve", "tensor_tensor", e[:, :], e[:, :], pM[:, :], ALU.mult, R=[bpM], W=[be])
                                P.I("pe", "matmul", pSm[:, :], onesb[:, :], e[:, :], start=first, stop=last, R=[be, b_const], W=[bpSm])
                                P.I("pe", "matmul", pO[:, :], vv[:, kb, :], e[:, :], start=first, stop=last, R=[be, bvv], W=[bpO])
                            rv, brv = OF.next()
                            P.I("dve", "tensor_scalar", rv[:, :], pSm[:, :], 1e-20, None, ALU.max, R=[bpSm], W=[brv])
                            P.I("dve", "reciprocal", rv[:, :], rv[:, :], W=[brv])
                            gs, bgs = gate_bc(br, h, cs)
                            P.I("pool", "tensor_tensor", rv[:, :], rv[:, :], gs[:, :], ALU.mult, R=[bgs], W=[brv])
                            t_, bt = OF.next()
                            P.I("dve", "tensor_tensor", t_[:, :], pO[:, :], rv[:, :], ALU.mult, R=[bpO, brv], W=[bt])
                            P.I("pool", "tensor_tensor", OACC[r][:, cs], OACC[r][:, cs], t_[:, :], ALU.add, R=[bt], W=[bOA[r]])

                tmap_n = {0: 0, -128: 1, -256: 2, -384: 3, 128: 4}
                tmap_w = {0: 0, -128: 1, -256: 2, -384: 3, 128: 4, 256: 5, 384: 6, 512: 7}
                for g in range(2):
                    P.dma("sp", kcT[:, :], KC[g], R=[bd["KC"]], W=[bkc])
                    P.dma("sp", vcT[:, :], VC[g], R=[bd["VC"]], W=[bvc])
                    for (srcT, bsrc, wname, posc, is_k) in ((kcT, bkc, "nsa_cmp_wk", V_PK, True), (vcT, bvc, "nsa_cmp_wv", V_PV, False)):
                        w, bw = WC.next()
                        P.dma("pool", w[:, :, :], dr[f"{wname}@{l}"].rearrange("(l d) o -> d l o", d=128), W=[bw])
                        pc, bpc = bankA()
                        sv = srcT[:, :].rearrange("p (c s) -> p c s", s=16)
                        for l_ in range(32):
                            a_, r_ = divmod(l_, 16)
                            tl, btl = TL.next()
                            P.I("dve", "tensor_scalar", tl[:, 0:127], sv[:, a_:a_ + 127, r_], VEC[:, posc + l_:posc + l_ + 1], None, ALU.add,
                                R=[bsrc, b_vec], W=[btl])
                            P.I("pe", "matmul", pc[0:127, 0:128], tl[:, 0:127], w[:, l_, :], start=(l_ == 0), stop=(l_ == 31),
                                R=[btl, bw], W=[bpc])
                        if is_k:
                            jk, bj = OF.next()
                            P.I("act", "activation", jk[0:127, 0:128], pc[0:127, 0:128], AF.Square, accum_out=ssq[0:127, 0:1],
                                R=[bpc], W=[bj, bss])
                            P.I("act", "activation", ssq[0:127, :], ssq[0:127, :], AF.Ln, bias=epst[0:127, 0:1], scale=1.0 / 128,
                                R=[b_const], W=[bss])
                            P.I("act", "activation", ssq[0:127, :], ssq[0:127, :], AF.Exp, scale=-0.5, W=[bss])
                            P.I("dve", "tensor_scalar", kn[0:127, :], pc[0:127, 0:128], ssq[0:127, 0:1], None, ALU.mult,
                                R=[bpc, bss], W=[bkn])
                            pT, bpT = bankA()
                            P.I("pe", "transpose", pT[:, 0:128], kn[:, :], ident[:, :], R=[bkn, b_const], W=[bpT])
                            P.I("dve", "tensor_scalar", kcbT[:, :], pT[:, 0:128], VEC[:, V_KCG:V_KCG + 1], None, ALU.mult,
                                R=[bpT, b_vec], W=[bkcb])
                        else:
                            P.I("act", "activation", vcb[0:127, :], pc[0:127, 0:128], AF.Copy, R=[bpc], W=[bvcb])
                    for qc in range(NTC):
                        cs = slice(qc * 512, (qc + 1) * 512)
                        for r in range(4):
                            h = 4 * g + r
                            qh, bqh = QH.next()
                            P.dma("sp", qh[:, :], QN[h][:, cs], R=[bd["QN"]], W=[bqh])
                            bC, bbC = BC.next()
                            P.dma("sp", bC[:, :], dr["k_biasC"][h][:, cs], W=[bbC])
                            pS, bpS = bankA()
                            P.I("pe", "matmul", pS[0:127, :], kcbT[:, 0:127], qh[:, :], start=True, stop=True, R=[bkcb, bqh], W=[bpS])
                            lg, blg = OF.next()
                            P.I("dve", "scalar_tensor_tensor", lg[0:127, :], pS[0:127, :], SCALE, bC[0:127, :], ALU.mult, ALU.add,
                                R=[bpS, bbC], W=[blg])
                            P.I("act", "activation", P4[0:127, r, :], lg[0:127, :], AF.Exp, R=[blg], W=[bP4[r]])
                            pSm, bpSm = bankA()
                            P.I("pe", "matmul", pSm[:, :], ones[0:127, :], P4[0:127, r, :], start=True, stop=True,
                                R=[bP4[r], b_const], W=[bpSm])
                            rv, brv = OF.next()
                            P.I("dve", "tensor_scalar", rv[:, :], pSm[:, :], 1e-20, None, ALU.max, R=[bpSm], W=[brv])
                            P.I("dve", "reciprocal", rv[:, :], rv[:, :], W=[brv])
                            P.I("pool", "tensor_tensor", P4[0:127, r, :], P4[0:127, r, :], rv[0:127, :], ALU.mult, R=[brv], W=[bP4[r]])
                            pO, bpO = bankA()
                            P.I("pe", "matmul", pO[:, :], vcb[0:127, :], P4[0:127, r, :], start=True, stop=True,
                                R=[bvcb, bP4[r]], W=[bpO])
                            gs, bgs = gate_bc(0, h, cs)
                            P.I("dve", "tensor_tensor", OACC[r][:, cs], pO[:, :], gs[:, :], ALU.mult, R=[bpO, bgs], W=[bOA[r]])
                        for qs in range(4):
                            qb = qc * 4 + qs
                            pI, bpI = bankA()
                            for r in range(4):
                                P.I("pe", "matmul", pI[:, 0:32], P4[0:127, r, qs * 128:(qs + 1) * 128], ovl[0:127, :],
                                    start=(r == 0), stop=(r == 3), R=[bP4[r], b_c2], W=[bpI])
                            sc_, bsc = SC.next()
                            P.I("dve", "tensor_tensor", sc_[:, :], pI[:, 0:32], cnf[:, qb, :], ALU.mult, R=[bpI, b_c2], W=[bsc])
                            P.I("dve", "tensor_tensor", sc_[:, :], sc_[:, :], addc[:, qb, :], ALU.add, R=[b_c2], W=[bsc])
                            P.I("dve", "max", m1[:, :], sc_[:, :], R=[bsc], W=[btk])
                            P.I("dve", "match_replace", sc2[:, :], m1[:, :], sc_[:, :], -1e30, R=[bsc], W=[btk])
                            P.I("dve", "max", m2[:, :], sc2[:, :], W=[btk])
                            P.I("dve", "tensor_scalar", sel[:, :], sc_[:, :], m2[:, 7:8], None, ALU.is_ge, R=[bsc], W=[btk])
                            P.I("dve", "scalar_tensor_tensor", sel[:, :], sc_[:, :], -5000.0, sel[:, :], ALU.is_gt, ALU.mult,
                                R=[bsc], W=[btk])
                            pT, bpT = bankA()
                            P.I("pe", "transpose", pT[0:32, 0:128], sel[:, 0:32], ident[:, :], R=[btk, b_const], W=[bpT])
                            P.I("act", "activation", SELT[0:32, qb * 128:(qb + 1) * 128], pT[0:32, 0:128], AF.Copy,
                                R=[bpT], W=[bSEL])
                    if "SELD" in debug:
                        P.dma("sp", SELD[g], SELT[0:32, :], R=[bSEL], A=[bd["SELD"]])
                    attn_branch(g, KS, bd["KS"], VS, bd["VS"], "k_biasN", 5, tmap_n, 1, True,
                                lambda qc: list(range(4 * qc + 3, -1, -1)))
                    attn_branch(g, KW, bd["KW"], VW, bd["VW"], "k_biasW", 8, tmap_w, 2, False,
                                lambda qc: list(range(4 * qc + 3, max(0, 4 * qc - 4) - 1, -1)))
                    for r in range(4):
                        h = 4 * g + r
                        for qc in range(NTC):
                            cs = slice(qc * 512, (qc + 1) * 512)
                            o, bo = EB.next()
                            P.I("act", "activation", o[:, :], OACC[r][:, cs], AF.Copy, R=[bOA[r]], W=[bo])
                            P.dma("sp", ONSA[h][:, cs], o[:, :], R=[bo], A=[bd["ONSA"]])
                end_phase()

        def make_resid(ph, ga_col, t0):
            XR = Rot([sb(f"rs_x{i}", [128, 512], F32, ph) for i in range(3)], "rsx")

            def epi(c, msz, tcn, pt, bp):
                dc = c // 128
                cols = slice(t0 + tcn * 512, t0 + (tcn + 1) * 512)
                xr, bx = XR.next()
                P.dma("sp", xr[:, :], XT[dc][:, cols], R=[bd["XT"]], W=[bx])
                P.I("dve", "scalar_tensor_tensor", xr[:, :], pt[:, :], MOD[:, ga_col + dc:ga_col + dc + 1], xr[:, :], ALU.mult, ALU.add,
                    R=[bp, b_mod], W=[bx])
                P.dma("sp", XT[dc][:, cols], xr[:, :], R=[bx], A=[bd["XT"]])
            return epi

        def out_phase(l):
            TS = 1024
            for sc_ in range(T // TS):
                t0 = sc_ * TS
                with nc.Block(), ExitStack() as ph:
                    IN = []
                    bIN = []
                    for bi, (src, bsrc) in enumerate(((YLN, bd["YLN"]), (OSB, bd["OSB"]), (ONSA, bd["ONSA"]))):
                        t_ = sb(f"op_in{bi}", [128, 8, TS], BF16, ph)
                        b_ = Buf(f"op_in{bi}")
                        P.dma("sp", t_[:, :, :], src.rearrange("c p t -> p c t")[:, :, t0:t0 + TS], R=[bsrc], W=[b_])
                        IN.append(t_)
                        bIN.append(b_)
                    MT = sb("op_mt", [128, 16, TS], BF16, ph)
                    bMT = [Buf(f"mt{i}") for i in range(TS // 512)]
                    WO = Rot([sb(f"op_w{i}", [128, 8, 512], BF16, ph) for i in range(3)], "opw")
                    GMT = Rot([sb(f"op_g{i}", [128, 512], BF16, ph) for i in range(3)], "opg")
                    ACC = Rot([sb(f"op_acc{i}", [128, 512], F32, ph) for i in range(2)], "opacc")
                    T2 = Rot([sb(f"op_t2{i}", [128, 512], F32, ph) for i in range(2)], "opt2")
                    names = ("w_conv_out", "w_sb_out", "w_nsa_out")
                    for dcg in range(4):
                        wts = [load_w(dr[f"{nm}@{l}"][:, dcg * 512:(dcg + 1) * 512], 8, 512, rot=WO) for nm in names]
                        for dci in range(4):
                            dc = dcg * 4 + dci
                            for tcn in range(TS // 512):
                                ts_ = slice(tcn * 512, (tcn + 1) * 512)
                                acc, bacc = ACC.next()
                                for bi in range(3):
                                    wt, bw = wts[bi]
                                    pY, bpY = bankA()
                                    for cc in range(8):
                                        P.I("pe", "matmul", pY[:, :], wt[:, cc, dci * 128:(dci + 1) * 128], IN[bi][:, cc, ts_],
                                            start=(cc == 0), stop=(cc == 7), R=[bw, bIN[bi]], W=[bpY], sig=(cc == 7))
                                    gm, bgm = GMT.next()
                                    P.dma("sp", gm[:, :], GM[bi * 16 + dc][:, t0 + tcn * 512:t0 + (tcn + 1) * 512], R=[bd["GM"]], W=[bgm])
                                    if bi == 0:
                                        P.I("dve", "tensor_tensor", acc[:, :], pY[:, :], gm[:, :], ALU.mult, R=[bpY, bgm], W=[bacc])
                                    else:
                                        t2, bt2 = T2.next()
                                        P.I("dve", "tensor_tensor", t2[:, :], pY[:, :], gm[:, :], ALU.mult, R=[bpY, bgm], W=[bt2])
                                        P.I("dve", "tensor_tensor", acc[:, :], acc[:, :], t2[:, :], ALU.add, R=[bt2], W=[bacc])
                                P.I("act", "activation", MT[:, dc, ts_], acc[:, :], AF.Copy, R=[bacc], A=[bMT[tcn]])
                    proj_fm(dr[f"w_o@{l}"], 16, 0, D, lambda kc, tcn: (MT[:, kc, tcn * 512:(tcn + 1) * 512], [bMT[tcn]]),
                            TS // 512, make_resid(ph, 32, t0))
                    end_phase()

        def ffn_phase(l):
            i = l // 2
            moe = (l % 2 == 1)
            TS = 1024
            for sc_ in range(T // TS):
                t0 = sc_ * TS
                with ExitStack() as fs:
                    HT2 = sb("f_HT", [128, 16, TS], BF16, fs)
                    cur["HT"] = HT2
                    CT = sb("f_CT", [8, TS], F32, fs) if moe else None
                    bCT = Buf("CT")
                    with nc.Block(), ExitStack() as ph:
                        cb = None
                        if moe:
                            H32 = sb("f_h32", [128, 16, 512], F32, ph)
                            bH32 = Buf("h32")
                            wr = sb("f_wr", [128, 16, NE], F32, ph)
                            rbt = sb("f_rb", [128, NE], F32, ph)
                            bwr = Buf("wr")
                            P.dma("sp", wr[:, :, :], dr[f"moe_router@{i}"].rearrange("(kc p) e -> p kc e", p=128), W=[bwr])
                            P.dma("sp", rbt[:, :], rb_d[i], A=[bwr])
                            lgt = sb("f_lgt", [128, NE], F32, ph)
                            mx = sb("f_mx", [128, 8], F32, ph)
                            sm = sb("f_sm", [128, 4], F32, ph)
                            ex = sb("f_ex", [128, NE], F32, ph)
                            cmb = sb("f_cmb", [128, NE], F32, ph)
                            brt = Buf("router")

                            def cb(tcn, kc, t_, bt):
                                if kc is not None:
                                    P.I("act", "activation", H32[:, kc, :], t_[:, :], AF.Identity, bias=MOD[:, 48 + kc:48 + kc + 1],
                                        scale=AFFN[:, kc:kc + 1], R=[bt, b_mod], A=[bH32])
                                    return
                                for qs in range(4):
                                    pR, bpR = bankA()
                                    for kc2 in range(16):
                                        P.I("pe", "matmul", pR[:, 0:NE], H32[:, kc2, qs * 128:(qs + 1) * 128], wr[:, kc2, :],
                                            start=(kc2 == 0), stop=(kc2 == 15), R=[bH32, bwr], W=[bpR], sig=(kc2 == 15))
                                    P.I("dve", "tensor_tensor", lgt[:, :], pR[:, 0:NE], rbt[:, :], ALU.add, R=[bpR, bwr], W=[brt])
                                    P.I("dve", "max", mx[:, :], lgt[:, :], W=[brt])
                                    P.I("dve", "tensor_scalar", sm[:, 0:1], mx[:, 0:1], -1.0, None, ALU.mult, W=[brt])
                                    P.I("act", "activation", ex[:, :], lgt[:, :], AF.Exp, bias=sm[:, 0:1], scale=1.0, W=[brt])
                                    P.I("act", "activation", sm[:, 1:2], mx[:, 1:2], AF.Exp, bias=sm[:, 0:1], scale=1.0, W=[brt])
                                    P.I("dve", "tensor_scalar", sm[:, 1:2], sm[:, 1:2], 1.0, None, ALU.add, W=[brt])
                                    P.I("dve", "reciprocal", sm[:, 2:3], sm[:, 1:2], W=[brt])
                                    P.I("dve", "tensor_scalar", cmb[:, :], lgt[:, :], mx[:, 1:2], None, ALU.is_ge, W=[brt])
                                    P.I("dve", "scalar_tensor_tensor", cmb[:, :], ex[:, :], sm[:, 2:3], cmb[:, :], ALU.mult, ALU.mult, W=[brt])
                                    pT, bpT = bankA()
                                    P.I("pe", "transpose", pT[0:NE, 0:128], cmb[:, 0:NE], ident[:, :], R=[brt, b_const], W=[bpT])
                                    c0 = tcn * 512 + qs * 128
                                    P.I("act", "activation", CT[0:NE, c0:c0 + 128], pT[0:NE, 0:128], AF.Copy, R=[bpT], A=[bCT])
                                bH32.w = dict(bH32.w)
                        norm_phase(ph, t0, TS, AFFN, 48, h32cb=cb)
                        end_phase()
                    with nc.Block(), ExitStack() as ph:
                        nfc = (DFE if moe else DFF) // 128
                        AT = sb("f_AT", [128, nfc, TS], BF16, ph)
                        bAT = [Buf(f"AT{t}") for t in range(TS // 512)]
                        SL = Rot([sb(f"f_s{k}", [128, 512], F32, ph) for k in range(3)], "fs")
                        W2 = Rot([sb(f"f_w2{k}", [128, nfc, 128], BF16, ph) for k in range(2)], "fw2")
                        resid = make_resid(ph, 80, t0)
                        sele = None
                        if moe:
                            sele = sb("f_sele", [8, 8, 128], F32, ph)
                            P.dma("sp", sele[:, :, :], dr["k_sele"], W=[b_c2])
                            cbs = [sb(f"f_cb{t}", [128, 512], F32, ph) for t in range(TS // 512)]
                            bcbs = [Buf("cb") for t in range(TS // 512)]
                        for e_ in range(NE if moe else 1):
                            if moe:
                                w1d, w3d, w2d = dr[f"moe_w1@{i}"][e_], dr[f"moe_w3@{i}"][e_], dr[f"moe_w2@{i}"][e_]
                                dff = DFE
                                for tcn in range(TS // 512):
                                    pC, bpC = bankA()
                                    P.I("pe", "matmul", pC[:, :], sele[:, e_, :], CT[0:NE, tcn * 512:(tcn + 1) * 512], start=True, stop=True,
                                        R=[b_c2, bCT], W=[bpC])
                                    P.I("act", "activation", cbs[tcn][:, :], pC[:, :], AF.Copy, R=[bpC], W=[bcbs[tcn]])
                            else:
                                w1d, w3d, w2d = dr[f"ffn_w1@{i}"], dr[f"ffn_w3@{i}"], dr[f"ffn_w2@{i}"]
                                dff = DFF
                            c = 0
                            while c < dff:
                                n = min(256, dff - c)
                                w1, bw1 = load_w(w1d[:, c:c + n], 16, n)
                                w3, bw3 = load_w(w3d[:, c:c + n], 16, n)
                                for m in range(0, n, 128):
                                    fc = (c + m) // 128
                                    for tcn in range(TS // 512):
                                        ts_ = slice(tcn * 512, (tcn + 1) * 512)
                                        p1, bp1 = bankA()
                                        p3, bp3 = bankA()
                                        for kc in range(16):
                                            P.I("pe", "matmul", p1[:, :], w1[:, kc, m:m + 128], HT2[:, kc, ts_], start=(kc == 0), stop=(kc == 15),
                                                R=[bw1, b_HT[tcn]], W=[bp1], sig=(kc == 15))
                                        for kc in range(16):
                                            P.I("pe", "matmul", p3[:, :], w3[:, kc, m:m + 128], HT2[:, kc, ts_], start=(kc == 0), stop=(kc == 15),
                                                R=[bw3, b_HT[tcn]], W=[bp3], sig=(kc == 15))
                                        s_, bs_ = SL.next()
                                        P.I("act", "activation", s_[:, :], p1[:, :], AF.Silu, R=[bp1], W=[bs_])
                                        if moe:
                                            P.I("dve", "tensor_tensor", s_[:, :], s_[:, :], p3[:, :], ALU.mult, R=[bp3], W=[bs_])
                                            P.I("dve", "tensor_tensor", AT[:, fc, ts_], s_[:, :], cbs[tcn][:, :], ALU.mult,
                                                R=[bs_, bcbs[tcn]], A=[bAT[tcn]])
                                        else:
                                            P.I("dve", "tensor_tensor", AT[:, fc, ts_], s_[:, :], p3[:, :], ALU.mult, R=[bs_, bp3], A=[bAT[tcn]])
                                c += n
                            nf = dff // 128
                            for dc in range(16):
                                w2, bw2 = W2.next()
                                P.dma("pool", w2[:, 0:nf, :], w2d[:, dc * 128:(dc + 1) * 128].rearrange("(kc p) n -> p kc n", p=128), W=[bw2])
                                for tcn in range(TS // 512):
                                    pY, bpY = bankA()
                                    for fc in range(nf):
                                        P.I("pe", "matmul", pY[:, :], w2[:, fc, :], AT[:, fc, tcn * 512:(tcn + 1) * 512],
                                            start=(fc == 0), stop=(fc == nf - 1), R=[bw2, bAT[tcn]], W=[bpY], sig=(fc == nf - 1))
                                    resid(dc * 128, 128, tcn, pY, bpY)
                            for b_ in bAT:
                                b_.w = dict(b_.w)
                        end_phase()

        for l in range(nlayers):
            if stop == "p0":
                break
            adaln(l)
            if stop == "adaln":
                break
            with ExitStack() as hs:
                cur["HT"] = sb("HTm", [128, 16, T], BF16, hs)
                with nc.Block(), ExitStack() as ph:
                    norm_phase(ph, 0, T, AMIX, 0)
                    end_phase()
                if stop == "norm":
                    break
                inproj_phase(l)
            if stop == "inproj":
                break
            sb_phase()
            if stop == "sb":
                break
            nsa_phase(l)
            if stop == "nsa":
                break
            out_phase(l)
            if stop == "out":
                break
            ffn_phase(l)

        with nc.Block(), ExitStack() as ph:
            fin = Rot([sb(f"f_in{i}", [128, 16, 128], F32, ph) for i in range(2)], "f_in")
            fo = Rot([sb(f"f_o{i}", [128, D], F32, ph) for i in range(2)], "f_o")
            XTv = XT.rearrange("c p t -> p c t")
            for tb in range(T // 128):
                xi, bxi = fin.next()
                P.dma("sp", xi[:, :, :], XTv[:, :, tb * 128:(tb + 1) * 128], R=[bd["XT"]], W=[bxi])
                o, bo = fo.next()
                for g4 in range(4):
                    pt, bp = bank()
                    for j in range(4):
                        P.I("pe", "transpose", pt[:, j * 128:(j + 1) * 128], xi[:, g4 * 4 + j, :], ident[:, :],
                            R=[bxi, b_const], W=[bp], sig=(j == 3))
                    if g4 % 2 == 0:
                        P.I("act", "activation", o[:, g4 * 512:(g4 + 1) * 512], pt[:, :], AF.Copy, R=[bp], A=[bo])
                    else:
                        P.I("dve", "tensor_copy", o[:, g4 * 512:(g4 + 1) * 512], pt[:, :], R=[bp], A=[bo])
                P.dma("sp", out_d[tb * 128:(tb + 1) * 128, :], o[:, :], R=[bo], A=[bd["out"]])
            P.drain_dmas()
        print("instructions", P.n_ins, "waits", P.n_wait)
    return nc, dr


def _vecs(inp):
    v = np.zeros((L, 128, NV), np.float32)

    def pm(a, n):
        return np.ascontiguousarray(a.reshape(n, 128).T)
    for l in range(L):
        v[l, :, V_BADA:V_BADA + 96] = pm(inp["b_ada"][l], 96)
        v[l, :, V_GMIX:V_GMIX + 16] = pm(inp["g_mix"][l], 16)
        v[l, :, V_GFFN:V_GFFN + 16] = pm(inp["g_ffn"][l], 16)
        v[l, :, V_CB:V_CB + 8] = pm(inp["conv_b"][l], 8)
        v[l, :, V_LNG:V_LNG + 8] = pm(inp["conv_ln_g"][l], 8)
        v[l, :, V_LNB:V_LNB + 8] = pm(inp["conv_ln_b"][l], 8)
        cw = inp["conv_w"][l]
        v[l, :, V_CW:V_CW + 248] = cw.reshape(31, 8, 128).transpose(2, 1, 0).reshape(128, 248)
        v[l, :, V_QG] = inp["nsa_q_g"][l]
        v[l, :, V_KCG] = inp["nsa_kc_g"][l]
        v[l, :, V_KSG] = inp["nsa_ks_g"][l]
        v[l, :, V_KWG] = inp["nsa_kw_g"][l]
        v[l, :, V_PK:V_PK + 32] = inp["nsa_cmp_pos_k"][l].T
        v[l, :, V_PV:V_PV + 32] = inp["nsa_cmp_pos_v"][l].T
    return v


def prep_shared(inp):
    sh = {}
    for k in WEIGHT_SHAPES:
        if k in inp:
            a = np.asarray(inp[k], dtype=np.float32)
            for li in range(a.shape[0]):
                sh[f"{k}_{li}"] = a[li]
    sh["vecs"] = _vecs(inp)
    sh["rbias"] = np.ascontiguousarray(np.broadcast_to(np.asarray(inp["moe_router_b"], np.float32)[:, None, :], (2, 128, NE)))
    for k, a in _host_consts(np.asarray(inp["rel_bias"], np.float32)).items():
        sh["k_" + k] = np.ascontiguousarray(a)
    return sh


def prep_core(inp, b, T=2048):
    d = {}
    d["x"] = np.ascontiguousarray(np.asarray(inp["x"][b], np.float32)[:T])
    d["cT"] = np.ascontiguousarray(np.asarray(inp["c"][b], np.float32).reshape(16, 128).T)
    return d


N_LAYERS_IMPL = L
STOP_AT = None
N_CORES = 4


def kernel(**inputs):
    T = 2048
    nc, dr = build(T=T, nlayers=N_LAYERS_IMPL, stop=STOP_AT)
    names = set()
    for k in dr:
        ap = dr[k]
        names.add(ap.tensor.name)
    sh = prep_shared(inputs)
    in_maps = []
    for core in range(N_CORES):
        b = core % 4
        m = dict(sh)
        m.update(prep_core(inputs, b, T))
        in_maps.append({k: np.ascontiguousarray(v) for k, v in m.items() if k in names})
    res = run_bass_kernel_spmd(nc, in_maps, core_ids=list(range(N_CORES)))
    out = np.stack([np.asarray(res.results[b]["out"], dtype=np.float32) for b in range(4)], 0)
    return out
```
